# Optimizing a Trainium2 kernel written in Bass

```python
import jax, jax.numpy as jnp
from jax import lax
import numpy as np

D_MODEL = 2048
BATCH = 4
SEQ = 4096
DEPTH = 2

GRID_W = 64
CTX_LEN = 256
HEAD_DIM = 128
N_GROUPS = 4
GROUP_W = D_MODEL // N_GROUPS
MIX_W = N_GROUPS * GROUP_W
ML_HEADS = GROUP_W // HEAD_DIM
ML_CHUNK = 128
N_GATE = 2 * 2 * ML_HEADS
SG_CHUNK = 128
SG_GROUPS = 4
SG_CH = GROUP_W // SG_GROUPS
CONV_W = 31
ATT_Q_HEADS = GROUP_W // HEAD_DIM
ATT_KV_HEADS = ATT_Q_HEADS // 2
ATT_BLOCK = 128
ROPE_THETA = 10000.0
AXIS_DIM = HEAD_DIM // 2
D_FF = 256 * ((8 * D_MODEL // 3 + 255) // 256)
FFN_CONV_W = 3
EPS = 1e-6
IN_SPLITS = (GROUP_W, GROUP_W, GROUP_W, GROUP_W, N_GATE,
             GROUP_W, GROUP_W,
             GROUP_W, GROUP_W,
             ATT_Q_HEADS * HEAD_DIM, ATT_KV_HEADS * HEAD_DIM, ATT_KV_HEADS * HEAD_DIM)
D_IN = sum(IN_SPLITS)

kernel_name = 'hybrid_parallel_groups_diffusion_block'


def rmsnorm(x, g):
    xf = x.astype(jnp.float32)
    y = xf * lax.rsqrt(jnp.mean(xf * xf, axis=-1, keepdims=True) + EPS)
    return (y * g.astype(jnp.float32)).astype(x.dtype)


def layernorm(x, g, b):
    xf = x.astype(jnp.float32)
    mu = jnp.mean(xf, axis=-1, keepdims=True)
    var = jnp.mean(jnp.square(xf - mu), axis=-1, keepdims=True)
    y = (xf - mu) * lax.rsqrt(var + EPS)
    return (y * g.astype(jnp.float32) + b.astype(jnp.float32)).astype(x.dtype)


def modulate(x, shift, scale):
    return x * (1.0 + scale) + shift


def split_cols(z):
    points = np.cumsum(IN_SPLITS)[:-1].tolist()
    return jnp.split(z, points, axis=-1)


def dwconv(x, w, b):
    y = lax.conv_general_dilated(x, w[:, None, :], window_strides=(1,), padding='SAME',
                                 dimension_numbers=('NWC', 'WIO', 'NWC'),
                                 feature_group_count=x.shape[-1])
    return y + b


def rope_2d(rows):
    t = jnp.arange(rows * GRID_W)
    row = (t // GRID_W).astype(jnp.float32)
    col = (t % GRID_W).astype(jnp.float32)
    inv = jnp.power(ROPE_THETA, -jnp.arange(0, AXIS_DIM, 2, dtype=jnp.float32) / AXIS_DIM)
    ang = jnp.concatenate([row[:, None] * inv, col[:, None] * inv], axis=-1)
    return jnp.cos(ang), jnp.sin(ang)


def apply_rope(x, cos, sin):
    half = x.shape[-1] // 2
    x1, x2 = x[..., :half], x[..., half:]
    c = cos[None, :, None, :].astype(x.dtype)
    s = sin[None, :, None, :].astype(x.dtype)
    return jnp.concatenate([x1 * c - x2 * s, x2 * c + x1 * s], axis=-1)


def mlstm_chunk_scan(q, k, v, li, lf, state):
    B, H, T, d = q.shape
    n_chunks = T // ML_CHUNK

    def to_chunks(a):
        return jnp.moveaxis(a.reshape((B, H, n_chunks, ML_CHUNK) + a.shape[3:]), 2, 0)

    tri = jnp.tril(jnp.ones((ML_CHUNK, ML_CHUNK), dtype=bool))

    def step(carry, inp):
        C, nv, m = carry
        qc, kc, vc, ic, fc = inp
        b = jnp.cumsum(fc, axis=-1)
        dmat = jnp.where(tri, b[..., :, None] - b[..., None, :] + ic[..., None, :], -jnp.inf)
        inter = b + m[..., None]
        mt = jnp.maximum(inter, jnp.max(dmat, axis=-1))
        w_intra = jnp.exp(dmat - mt[..., None])
        w_inter = jnp.exp(inter - mt)
        s = jnp.einsum('bhtd,bhsd->bhts', qc, kc) * w_intra
        num = w_inter[..., None] * jnp.einsum('bhtd,bhde->bhte', qc, C) + jnp.einsum('bhts,bhse->bhte', s, vc)
        den = w_inter * jnp.einsum('bhtd,bhd->bht', qc, nv) + jnp.sum(s, axis=-1)
        h = num / jnp.maximum(jnp.abs(den), jnp.exp(-mt))[..., None]
        b_last = b[..., -1]
        g = b_last[..., None] - b + ic
        m_new = jnp.maximum(b_last + m, jnp.max(g, axis=-1))
        a = jnp.exp(b_last + m - m_new)
        w = jnp.exp(g - m_new[..., None])
        C_new = a[..., None, None] * C + jnp.einsum('bhs,bhsd,bhse->bhde', w, kc, vc)
        n_new = a[..., None] * nv + jnp.einsum('bhs,bhsd->bhd', w, kc)
        return (C_new, n_new, m_new), h

    state, hs = lax.scan(step, state, (to_chunks(q), to_chunks(k), to_chunks(v), to_chunks(li), to_chunks(lf)))
    return jnp.moveaxis(hs, 0, 2).reshape(B, H, T, d), state


def mlstm_out(h, o, norm_g):
    B, H, T, d = h.shape
    mu = jnp.mean(h, axis=-1, keepdims=True)
    var = jnp.mean(jnp.square(h - mu), axis=-1, keepdims=True)
    hn = ((h - mu) * lax.rsqrt(var + EPS)).transpose(0, 2, 1, 3).reshape(B, T, H * d)
    hn = hn * norm_g.astype(jnp.float32)
    return (hn * jax.nn.sigmoid(o.astype(jnp.float32))).astype(o.dtype)


def mlstm_mixer(lat, ctx, gate_b, norm_g, need_ctx):
    def prep(parts):
        q, k, v, o, g = parts
        B, T, _ = q.shape

        def hd(a):
            return a.reshape(B, T, ML_HEADS, HEAD_DIM).transpose(0, 2, 1, 3).astype(jnp.float32)
        gg = (g + gate_b).astype(jnp.float32).reshape(B, T, 2, 2, ML_HEADS).transpose(2, 3, 0, 4, 1)
        return hd(q), hd(k) * (HEAD_DIM ** -0.5), hd(v), o, gg

    ql, kl, vl, ol, gl = prep(lat)
    qc, kc, vc, oc, gc = prep(ctx)
    B = qc.shape[0]
    state0 = (jnp.zeros((B, ML_HEADS, HEAD_DIM, HEAD_DIM), jnp.float32),
              jnp.zeros((B, ML_HEADS, HEAD_DIM), jnp.float32),
              jnp.zeros((B, ML_HEADS), jnp.float32))
    h_l = jnp.zeros_like(ql)
    h_c = jnp.zeros_like(qc)
    for direction in range(2):
        if direction == 0:
            flip = lambda a: a
        else:
            flip = lambda a: jnp.flip(a, axis=2)
        hc, st = mlstm_chunk_scan(flip(qc), flip(kc), flip(vc), flip(gc[direction, 0]),
                                  flip(jax.nn.log_sigmoid(gc[direction, 1])), state0)
        hl, _ = mlstm_chunk_scan(flip(ql), flip(kl), flip(vl), flip(gl[direction, 0]),
                                 flip(jax.nn.log_sigmoid(gl[direction, 1])), st)
        h_l = h_l + flip(hl)
        if need_ctx:
            h_c = h_c + flip(hc)
    y_l = mlstm_out(h_l, ol, norm_g)
    y_c = mlstm_out(h_c, oc, norm_g) if need_ctx else None
    return y_l, y_c


def spatial_gating(zu, zv, ln_g, ln_b, w_s, b_s):
    u = jax.nn.gelu(zu)
    v = layernorm(jax.nn.gelu(zv), ln_g, ln_b)
    B, T, _ = u.shape
    vg = v.reshape(B, T // SG_CHUNK, SG_CHUNK, SG_GROUPS, SG_CH)
    mixed = jnp.einsum('gts,bnsgc->bntgc', w_s, vg) + b_s.T[None, None, :, :, None]
    return u * mixed.reshape(B, T, GROUP_W)


def conformer_conv(za, zg, w_dw, b_dw, ln_g, ln_b):
    y = za * jax.nn.sigmoid(zg)
    y = dwconv(y, w_dw, b_dw)
    return jax.nn.silu(layernorm(y, ln_g, ln_b))


def attend(q, k, v):
    B, Tq, Hq, d = q.shape
    Hkv = k.shape[2]
    G = Hq // Hkv
    nb = Tq // ATT_BLOCK
    qb = jnp.moveaxis(q.reshape(B, nb, ATT_BLOCK, Hkv, G, d), 1, 0)
    scale = d ** -0.5

    def one(qblk):
        s = jnp.einsum('bqhgd,bkhd->bhgqk', qblk, k).astype(jnp.float32) * scale
        p = jax.nn.softmax(s, axis=-1).astype(v.dtype)
        return jnp.einsum('bhgqk,bkhd->bqhgd', p, v)

    o = lax.map(one, qb)
    return jnp.moveaxis(o, 0, 1).reshape(B, Tq, Hq * d)


def conv_glu_ffn(h, w_up, w_dw, b_dw, w_down):
    g, u = jnp.split(h @ w_up, 2, axis=-1)
    return (jax.nn.silu(dwconv(g, w_dw, b_dw)) * u) @ w_down


def heads(a, n):
    return a.reshape(a.shape[0], a.shape[1], n, HEAD_DIM)


def hybrid_layer(x, xc, mod_l, mod_c, cos, sin, pre_mix_g, post_mix_g, w_in, ml_gate_b, ml_norm_g,
                 sg_ln_g, sg_ln_b, sg_w, sg_b, cv_w, cv_b, cv_ln_g, cv_ln_b, at_qn_g, at_kn_g, w_out,
                 pre_ffn_g, post_ffn_g, w_up, ffn_cv_w, ffn_cv_b, w_down, need_ctx):
    sh1, sc1, g1, sh2, sc2, g2 = jnp.split(mod_l, 6, axis=-1)
    sh1c, sc1c, g1c, sh2c, sc2c, g2c = jnp.split(mod_c, 6, axis=-1)

    zl = split_cols(modulate(rmsnorm(x, pre_mix_g), sh1, sc1) @ w_in)
    zc = split_cols(modulate(rmsnorm(xc, pre_mix_g), sh1c, sc1c) @ w_in)

    ya_l, ya_c = mlstm_mixer(zl[0:5], zc[0:5], ml_gate_b, ml_norm_g, need_ctx)
    yb_l = spatial_gating(zl[5], zl[6], sg_ln_g, sg_ln_b, sg_w, sg_b)
    yc_l = conformer_conv(zl[7], zl[8], cv_w, cv_b, cv_ln_g, cv_ln_b)

    q_l = apply_rope(rmsnorm(heads(zl[9], ATT_Q_HEADS), at_qn_g), cos, sin)
    k_l = apply_rope(rmsnorm(heads(zl[10], ATT_KV_HEADS), at_kn_g), cos, sin)
    v_l = heads(zl[11], ATT_KV_HEADS)
    k_c = rmsnorm(heads(zc[10], ATT_KV_HEADS), at_kn_g)
    v_c = heads(zc[11], ATT_KV_HEADS)
    yd_l = attend(q_l, jnp.concatenate([k_c, k_l], axis=1), jnp.concatenate([v_c, v_l], axis=1))

    y_l = jnp.concatenate([ya_l, yb_l, yc_l, yd_l], axis=-1) @ w_out
    x = x + g1 * rmsnorm(y_l, post_mix_g)

    h2 = modulate(rmsnorm(x, pre_ffn_g), sh2, sc2)
    x = x + g2 * rmsnorm(conv_glu_ffn(h2, w_up, ffn_cv_w, ffn_cv_b, w_down), post_ffn_g)

    if need_ctx:
        yb_c = spatial_gating(zc[5], zc[6], sg_ln_g, sg_ln_b, sg_w, sg_b)
        yc_c = conformer_conv(zc[7], zc[8], cv_w, cv_b, cv_ln_g, cv_ln_b)
        q_c = rmsnorm(heads(zc[9], ATT_Q_HEADS), at_qn_g)
        yd_c = attend(q_c, k_c, v_c)
        y_c = jnp.concatenate([ya_c, yb_c, yc_c, yd_c], axis=-1) @ w_out
        xc = xc + g1c * rmsnorm(y_c, post_mix_g)
        h2c = modulate(rmsnorm(xc, pre_ffn_g), sh2c, sc2c)
        xc = xc + g2c * rmsnorm(conv_glu_ffn(h2c, w_up, ffn_cv_w, ffn_cv_b, w_down), post_ffn_g)
    return x, xc


def setup_inputs(seed: int = 0) -> dict:
    key = jax.random.key(seed)
    ks = jax.random.split(key, 32)

    def nrm(k, shape, scale):
        return jax.random.normal(k, shape, jnp.float32) * scale

    def gain(k, n):
        return 1.0 + 0.05 * jax.random.normal(k, (DEPTH, n), jnp.float32)

    i_bias = 0.1 * jax.random.normal(ks[9], (DEPTH, 2, 1, ML_HEADS), jnp.float32)
    f_bias = 3.0 + 3.0 * jax.random.uniform(ks[10], (DEPTH, 2, 1, ML_HEADS), jnp.float32)
    ml_gate_b = jnp.concatenate([i_bias, f_bias], axis=2).reshape(DEPTH, N_GATE)
    return {
        'x': nrm(ks[0], (BATCH, SEQ, D_MODEL), 1.0),
        'c': nrm(ks[1], (BATCH, D_MODEL), 1.0),
        'ctx': nrm(ks[2], (BATCH, CTX_LEN, D_MODEL), 1.0),
        'c_ctx': nrm(ks[3], (D_MODEL,), 1.0),
        'w_ada': nrm(ks[4], (DEPTH, D_MODEL, 6 * D_MODEL), 0.5 * D_MODEL ** -0.5),
        'b_ada': nrm(ks[5], (DEPTH, 6 * D_MODEL), 0.02),
        'pre_mix_g': gain(ks[6], D_MODEL),
        'post_mix_g': gain(ks[7], D_MODEL),
        'w_in': nrm(ks[8], (DEPTH, D_MODEL, D_IN), D_MODEL ** -0.5),
        'ml_gate_b': ml_gate_b,
        'ml_norm_g': gain(ks[11], GROUP_W),
        'sg_ln_g': gain(ks[12], GROUP_W),
        'sg_ln_b': nrm(ks[13], (DEPTH, GROUP_W), 0.02),
        'sg_w': nrm(ks[14], (DEPTH, SG_GROUPS, SG_CHUNK, SG_CHUNK), SG_CHUNK ** -0.5),
        'sg_b': 1.0 + 0.1 * jax.random.normal(ks[15], (DEPTH, SG_GROUPS, SG_CHUNK), jnp.float32),
        'cv_w': nrm(ks[16], (DEPTH, CONV_W, GROUP_W), CONV_W ** -0.5),
        'cv_b': nrm(ks[17], (DEPTH, GROUP_W), 0.02),
        'cv_ln_g': gain(ks[18], GROUP_W),
        'cv_ln_b': nrm(ks[19], (DEPTH, GROUP_W), 0.02),
        'at_qn_g': gain(ks[20], HEAD_DIM),
        'at_kn_g': gain(ks[21], HEAD_DIM),
        'w_out': nrm(ks[22], (DEPTH, MIX_W, D_MODEL), MIX_W ** -0.5),
        'pre_ffn_g': gain(ks[23], D_MODEL),
        'post_ffn_g': gain(ks[24], D_MODEL),
        'w_up': nrm(ks[25], (DEPTH, D_MODEL, 2 * D_FF), D_MODEL ** -0.5),
        'ffn_cv_w': nrm(ks[26], (DEPTH, FFN_CONV_W, D_FF), FFN_CONV_W ** -0.5),
        'ffn_cv_b': nrm(ks[27], (DEPTH, D_FF), 0.02),
        'w_down': nrm(ks[28], (DEPTH, D_FF, D_MODEL), D_FF ** -0.5),
    }


def reference(x, c, ctx, c_ctx, w_ada, b_ada, pre_mix_g, post_mix_g, w_in, ml_gate_b, ml_norm_g,
              sg_ln_g, sg_ln_b, sg_w, sg_b, cv_w, cv_b, cv_ln_g, cv_ln_b, at_qn_g, at_kn_g, w_out,
              pre_ffn_g, post_ffn_g, w_up, ffn_cv_w, ffn_cv_b, w_down):
    ROWS = x.shape[1] // GRID_W
    cos, sin = rope_2d(ROWS)
    s_lat = jax.nn.silu(c)
    s_ctx = jax.nn.silu(c_ctx)
    xc = ctx
    for i in range(DEPTH):
        mod_l = (s_lat @ w_ada[i] + b_ada[i])[:, None, :]
        mod_c = (s_ctx @ w_ada[i] + b_ada[i])[None, None, :]
        x, xc = hybrid_layer(x, xc, mod_l, mod_c, cos, sin, pre_mix_g[i], post_mix_g[i], w_in[i],
                             ml_gate_b[i], ml_norm_g[i], sg_ln_g[i], sg_ln_b[i], sg_w[i], sg_b[i],
                             cv_w[i], cv_b[i], cv_ln_g[i], cv_ln_b[i], at_qn_g[i], at_kn_g[i], w_out[i],
                             pre_ffn_g[i], post_ffn_g[i], w_up[i], ffn_cv_w[i], ffn_cv_b[i], w_down[i],
                             i < DEPTH - 1)
    return x
```

```python
import contextlib
import os
CUT = int(os.environ.get('CUTA', '99'))
import numpy as np
import concourse.bass as bass
import concourse.mybir as mybir
from concourse.bass_utils import run_bass_kernel_spmd

F32 = mybir.dt.float32
BF16 = mybir.dt.bfloat16
AF = mybir.ActivationFunctionType
ALU = mybir.AluOpType
AX = mybir.AxisListType

D = 2048
DIN = 5136
DFF = 5632
TC = 256
TL = 4096
T = TC + TL
NT = T // 128
EPS = 1e-6
KC = D // 128
FC = DFF // 128
NCORES = 8


class Buf:
    __slots__ = ("name", "w", "r")

    def __init__(self, name=""):
        self.name = name
        self.w = None
        self.r = {}


class Sched:
    KD = 8

    def __init__(self, nc):
        self.nc = nc
        self.E = {"pe": nc.tensor, "dve": nc.vector, "act": nc.scalar, "pool": nc.gpsimd, "sp": nc.sync}
        self.sem = {}
        self.cnt = {}
        for e in ["pe", "dve", "act", "pool"]:
            self.sem[e] = nc.alloc_semaphore("s_" + e)
            self.cnt[e] = 0
        self.dq = {}
        for q in ["sp", "act", "pool"]:
            self.dq[q] = {"n": 0, "sems": [nc.alloc_semaphore(f"d_{q}{i}") for i in range(self.KD)],
                          "tot": [0] * self.KD}
            for i in range(self.KD):
                self.sem[(q, i)] = self.dq[q]["sems"][i]
        self.known = {e: {} for e in self.E}

    def _wait(self, eng, key, val):
        if val <= 0:
            return
        k = self.known[eng]
        if k.get(key, 0) >= val:
            return
        self.E[eng].wait_ge(self.sem[key], val)
        k[key] = val

    def _deps(self, eng, reads, writes):
        need = {}
        for b in reads:
            if b.w is not None:
                need[b.w[0]] = max(need.get(b.w[0], 0), b.w[1])
        for b in writes:
            if b.w is not None:
                need[b.w[0]] = max(need.get(b.w[0], 0), b.w[1])
            for k, v in b.r.items():
                need[k] = max(need.get(k, 0), v)
        for k, v in need.items():
            if k == "pe" and eng == "pe":
                continue
            self._wait(eng, k, v)

    def _mark(self, tok, reads, writes):
        for b in reads:
            b.r[tok[0]] = max(b.r.get(tok[0], 0), tok[1])
        for b in writes:
            b.w = tok
            b.r = {}

    def op(self, eng, emit, reads=(), writes=()):
        self._deps(eng, reads, writes)
        inst = emit(self.E[eng])
        self.cnt[eng] += 1
        inst.then_inc(self.sem[eng], 1)
        self._mark((eng, self.cnt[eng]), reads, writes)

    def dma(self, q, out, in_, reads=(), writes=(), **kw):
        self._deps(q, reads, writes)
        d = self.dq[q]
        i = d["n"] % self.KD
        d["n"] += 1
        self._wait(q, (q, i), d["tot"][i])
        self.E[q].dma_start(out=out, in_=in_, **kw).then_inc(d["sems"][i], 16)
        d["tot"][i] += 16
        self._mark(((q, i), d["tot"][i]), reads, writes)

    def barrier(self):
        tot = {}
        for e in ["pe", "dve", "act", "pool"]:
            tot[e] = self.cnt[e]
        for q in self.dq:
            for i in range(self.KD):
                tot[(q, i)] = self.dq[q]["tot"][i]
        for e in self.E:
            for k, v in tot.items():
                if k == e:
                    continue
                self._wait(e, k, v)


class Ring:
    def __init__(self, tiles):
        self.tiles = tiles
        self.i = 0

    def next(self):
        t = self.tiles[self.i % len(self.tiles)]
        self.i += 1
        return t


class K:
    def __init__(self, debug=None, nlayers=2, stop_after=None):
        self.debug = debug or ()
        self.nlayers = nlayers
        self.stop_after = stop_after
        nc = self.nc = bass.Bass("TRN2", target_bir_lowering=False)
        self.S = Sched(nc)
        self.uid = 0
        self.outs = []
        self.build()

    def name(self, p):
        self.uid += 1
        return f"{p}_{self.uid}"

    def din(self, name, shape, dt=F32):
        return self.nc.dram_tensor(name, list(shape), dt, kind="ExternalInput").ap()

    def scratch(self, name, shape, dt):
        if name in self.debug:
            self.outs.append(name)
            return self.nc.dram_tensor(name, list(shape), dt, kind="ExternalOutput").ap()
        return self.nc.dram_tensor(name, list(shape), dt, kind="Internal").ap()

    def sb(self, stack, p, shape, dt):
        t = stack.enter_context(self.nc.sbuf_tensor(self.name(p), list(shape), dt))
        return t, Buf(p)

    def ring(self, stack, p, shape, dt, n):
        return Ring([self.sb(stack, p, shape, dt) for _ in range(n)])

    def op(self, eng, emit, reads=(), writes=()):
        self.S.op(eng, emit, reads, writes)

    def load(self, out, in_, writes, q="sp", reads=()):
        self.S.dma(q, out, in_, reads=reads, writes=writes)

    def store(self, out, in_, reads, q="sp"):
        self.S.dma(q, out, in_, reads=reads, writes=())

    def load_fm(self, stack, dst_ap, dst_buf, src_rows_ap, R):
        st, bst = self.stage_fm
        self.load(st[0:R, :], src_rows_ap, [bst])
        ps, bps = self.PS[7]
        self.op("pe", lambda e: e.transpose(out=ps[:, 0:R], in_=st[0:R, :], identity=self.ident[0:R, 0:R]),
                reads=[bst, self.bconst], writes=[bps])
        self.op("dve", lambda e: e.tensor_copy(out=dst_ap, in_=ps[:, 0:R]), reads=[bps], writes=[dst_buf])

    def build(self):
        nc = self.nc
        S = self.S
        I = self.I = {}
        I["xin"] = self.din("xin", [T, D])
        I["cvec"] = self.din("cvec", [2, D])
        I["rope"] = self.din("rope", [TL, 128])
        I["w_ada"] = self.din("w_ada", [2, D, 6 * D])
        I["b_ada"] = self.din("b_ada", [2, 6 * D])
        for n, sh in [("pre_mix_g", [2, D]), ("post_mix_g", [2, D]), ("w_in", [2, D, DIN]), ("ml_gate_b", [2, 16]),
                      ("ml_norm_g", [2, 512]), ("sg_ln_g", [2, 512]), ("sg_ln_b", [2, 512]), ("sg_w", [2, 4, 128, 128]),
                      ("sg_b", [2, 4, 128]), ("cv_w", [2, 31, 512]), ("cv_b", [2, 512]), ("cv_ln_g", [2, 512]),
                      ("cv_ln_b", [2, 512]), ("at_qn_g", [2, 128]), ("at_kn_g", [2, 128]), ("w_out", [2, D, D]),
                      ("pre_ffn_g", [2, D]), ("post_ffn_g", [2, D]), ("w_up", [2, D, 2 * DFF]),
                      ("ffn_cv_w", [2, 3, DFF]), ("ffn_cv_b", [2, DFF]), ("w_down", [2, DFF, D])]:
            I[n] = self.din(n, sh)
        self.out = nc.dram_tensor("out", [TL, D], F32, kind="ExternalOutput").ap()

        Z = self.Z = {}
        Z["XA"] = self.scratch("XA", [T, D], F32)
        Z["XB"] = self.scratch("XB", [T, D], F32)
        Z["MQT"] = self.scratch("MQT", [4, 128, T], BF16)
        Z["MKT"] = self.scratch("MKT", [4, 128, T], BF16)
        Z["MK"] = self.scratch("MK", [T, 512], BF16)
        Z["MV"] = self.scratch("MV", [T, 512], BF16)
        Z["MO"] = self.scratch("MO", [T, 512], BF16)
        Z["MG"] = self.scratch("MG", [T, 16], F32)
        Z["SU"] = self.scratch("SU", [4, 128, T], BF16)
        Z["SV"] = self.scratch("SV", [T, 512], BF16)
        Z["CY"] = self.scratch("CY", [4, 128, T], F32)
        Z["AQT"] = self.scratch("AQT", [4, 128, T], BF16)
        Z["AKT"] = self.scratch("AKT", [2, 128, T], BF16)
        Z["AV"] = self.scratch("AV", [T, 256], BF16)
        Z["YT"] = self.scratch("YT", [16, 128, T], BF16)
        Z["HF"] = self.scratch("HF", [T, 512], F32)
        Z["GV"] = self.scratch("GV", [2, 2, 2, D], F32)
        Z["RAW"] = self.scratch("RAW", [T, D], F32)

        with contextlib.ExitStack() as gs:
            self.PS = []
            for i in range(8):
                if i < 2:
                    t = gs.enter_context(nc.psum_tensor(f"ps{i}", [128, 1024], BF16))
                else:
                    t = gs.enter_context(nc.psum_tensor(f"ps{i}", [128, 512], F32))
                self.PS.append((t, Buf(f"ps{i}")))
            self.bconst = Buf("const")
            identf = gs.enter_context(nc.sbuf_tensor("identf", [128, 128], F32))
            identb = gs.enter_context(nc.sbuf_tensor("identb", [128, 128], BF16))
            onesf = gs.enter_context(nc.sbuf_tensor("onesf", [128, 128], F32))
            onesb = gs.enter_context(nc.sbuf_tensor("onesb", [128, 128], BF16))
            mkF = gs.enter_context(nc.sbuf_tensor("mkF", [128, 128], F32))
            mkB = gs.enter_context(nc.sbuf_tensor("mkB", [128, 128], F32))
            self.ident, self.identb, self.onesf, self.onesb, self.mkF, self.mkB = identf, identb, onesf, onesb, mkF, mkB
            bc = self.bconst
            self.op("pool", lambda e: e.memset(identf[:], 0.0), writes=[bc])
            self.op("pool", lambda e: e.affine_select(out=identf[:], in_=identf[:], pattern=[[-1, 128]],
                                                      compare_op=ALU.not_equal, fill=1.0, base=0, channel_multiplier=1),
                    reads=[bc], writes=[bc])
            self.op("pool", lambda e: e.memset(onesf[:], 1.0), writes=[bc])
            self.op("pool", lambda e: e.affine_select(out=mkF[:], in_=onesf[:], pattern=[[1, 128]],
                                                      compare_op=ALU.is_ge, fill=0.0, base=0, channel_multiplier=-1),
                    reads=[bc], writes=[bc])
            self.op("pool", lambda e: e.affine_select(out=mkB[:], in_=onesf[:], pattern=[[-1, 128]],
                                                      compare_op=ALU.is_ge, fill=0.0, base=0, channel_multiplier=1),
                    reads=[bc], writes=[bc])
            self.op("dve", lambda e: e.tensor_copy(out=identb[:], in_=identf[:]), reads=[bc], writes=[bc])
            self.op("dve", lambda e: e.tensor_copy(out=onesb[:], in_=onesf[:]), reads=[bc], writes=[bc])
            st = gs.enter_context(nc.sbuf_tensor("stage_fm", [128, 128], F32))
            self.stage_fm = (st, Buf("stage_fm"))
            self.mod = [[{}, {}] for _ in range(2)]
            self.bmod = Buf("mod")
            for l in range(2):
                for w in range(2):
                    for nm in ["gsc1", "sh1", "gsc2", "sh2"]:
                        self.mod[l][w][nm] = gs.enter_context(nc.sbuf_tensor(f"mod_{l}_{w}_{nm}", [128, KC], F32))

            for l in range(self.nlayers):
                self.phase0(l)
            if self.stop_after == "0":
                S.barrier()
                return
            for l in range(self.nlayers):
                last = (l == 1)
                xsrc = I["xin"] if l == 0 else Z["XB"]
                self.phaseA(l, xsrc, last)
                if self.stop_after == "A":
                    break
                stop = False
                for nm, fn in [("B", self.phaseB), ("C", self.phaseC), ("D", self.phaseD), ("E", self.phaseE)]:
                    only = os.environ.get("ONLY")
                    if only is None or nm in only:
                        fn(l, last)
                    if self.stop_after == nm:
                        stop = True
                        break
                if stop:
                    break
                self.phaseF(l, xsrc, last)
                if self.stop_after == "F":
                    break
                self.phaseG(l, last)
            S.barrier()

    def phase0(self, l):
        nc, S, I, Z = self.nc, self.S, self.I, self.Z
        with contextlib.ExitStack() as st:
            sstage, bss = self.sb(st, "sstage", [32, 128], F32)
            self.load(sstage[0:16, :], I["cvec"][0].rearrange("(k p) -> k p", p=128), [bss])
            self.load(sstage[16:32, :], I["cvec"][1].rearrange("(k p) -> k p", p=128), [bss])
            self.op("act", lambda e: e.activation(out=sstage[:], in_=sstage[:], func=AF.Silu), reads=[bss], writes=[bss])
            ps, bps = self.PS[7]
            self.op("pe", lambda e: e.transpose(out=ps[:, 0:32], in_=sstage[:], identity=self.ident[0:32, 0:32]),
                    reads=[bss, self.bconst], writes=[bps])
            sT, bsT = self.sb(st, "sT", [128, KC, 2], BF16)
            self.op("dve", lambda e: e.tensor_copy(out=sT[:].rearrange("p k w -> p w k"),
                                                   in_=ps[:, 0:32].rearrange("p (w k) -> p w k", w=2)),
                    reads=[bps], writes=[bsT])
            srep, bsr = self.sb(st, "srep", [128, 2, KC, 128], BF16)
            for w in range(2):
                self.op("dve", lambda e, w=w: e.tensor_copy(out=srep[:, w, :, :],
                                                            in_=sT[:, :, w:w + 1].to_broadcast([128, KC, 128])),
                        reads=[bsT], writes=[bsr])
            bada, bbada = self.sb(st, "bada", [1, 6 * D], F32)
            self.load(bada[:], I["b_ada"][l:l + 1, :], [bbada])
            badab, bbadab = self.sb(st, "badab", [1, 6 * D], BF16)
            self.op("dve", lambda e: e.tensor_copy(out=badab[:], in_=bada[:]), reads=[bbada], writes=[bbadab])
            gfm, bgfm = self.sb(st, "gfm", [128, 2, KC], F32)
            self.load_fm(st, gfm[:, 0, :], bgfm, I["pre_mix_g"][l].rearrange("(k p) -> k p", p=128), KC)
            self.load_fm(st, gfm[:, 1, :], bgfm, I["pre_ffn_g"][l].rearrange("(k p) -> k p", p=128), KC)
            pgb, bpgb = self.sb(st, "pgb", [128, 2, D], F32)
            self.load(pgb[:, 0, :], I["post_mix_g"][l].partition_broadcast(128), [bpgb])
            self.load(pgb[:, 1, :], I["post_ffn_g"][l].partition_broadcast(128), [bpgb])
            wring = self.ring(st, "wada", [128, KC, 512], BF16, 3)
            wsrc = I["w_ada"][l].rearrange("(k p) c -> p k c", p=128)
            pm, bpm = self.PS[6]
            gstage = self.ring(st, "gstage", [128, 512], F32, 2)
            for blk in range(24):
                seg = blk // 4
                wt, bwt = wring.next()
                self.load(wt[:], wsrc[:, :, blk * 512:(blk + 1) * 512], [bwt], q="pool")
                if seg in (2, 5):
                    gi = 0 if seg == 2 else 1
                    for w in range(2):
                        pg, bpg = self.PS[2 + (blk * 2 + w) % 4]

                        def em(e, w=w, pg=pg, wt=wt, blk=blk):
                            for kc in range(KC):
                                e.matmul(pg[:], lhsT=srep[:, w, kc, :], rhs=wt[:, kc, :], start=(kc == 0), stop=False)
                            return e.matmul(pg[:], lhsT=self.onesb[0:1, :], rhs=badab[0:1, blk * 512:(blk + 1) * 512],
                                            start=False, stop=True)
                        self.op("pe", em, reads=[bsr, bwt, bbadab, self.bconst], writes=[bpg])
                        gt, bgt = gstage.next()
                        c0 = (blk % 4) * 512
                        self.op("dve", lambda e, pg=pg, gt=gt, gi=gi, c0=c0: e.tensor_tensor(
                            out=gt[:], in0=pg[:], in1=pgb[:, gi, c0:c0 + 512], op=ALU.mult), reads=[bpg, bpgb], writes=[bgt])
                        self.store(Z["GV"][l, w, gi:gi + 1, c0:c0 + 512], gt[0:1, :], [bgt])
                else:
                    def em(e, wt=wt, blk=blk):
                        r = None
                        for j in range(4):
                            cc = blk * 4 + j
                            for kc in range(KC):
                                e.matmul(pm[:, cc * 2:cc * 2 + 2], lhsT=wt[:, kc, j * 128:(j + 1) * 128], rhs=sT[:, kc, :],
                                         start=(kc == 0), stop=False)
                            r = e.matmul(pm[:, cc * 2:cc * 2 + 2], lhsT=badab[0:1, cc * 128:(cc + 1) * 128],
                                         rhs=self.onesb[0:1, 0:2], start=False, stop=True)
                        return r
                    self.op("pe", em, reads=[bsT, bwt, bbadab, self.bconst], writes=[bpm])
            pmv = pm[:, 0:192].rearrange("p (c w) -> p c w", w=2)
            for w in range(2):
                m = self.mod[l][w]
                for nm, sc_seg, sh_seg, gi in [("1", 1, 0, 0), ("2", 4, 3, 1)]:
                    self.op("dve", lambda e, m=m, nm=nm, sc_seg=sc_seg, gi=gi, w=w: e.scalar_tensor_tensor(
                        out=m["gsc" + nm][:], in0=pmv[:, sc_seg * 16:(sc_seg + 1) * 16, w], scalar=1.0, in1=gfm[:, gi, :],
                        op0=ALU.add, op1=ALU.mult), reads=[bpm, bgfm], writes=[self.bmod])
                    self.op("dve", lambda e, m=m, nm=nm, sh_seg=sh_seg, w=w: e.tensor_copy(
                        out=m["sh" + nm][:], in_=pmv[:, sh_seg * 16:(sh_seg + 1) * 16, w]), reads=[bpm], writes=[self.bmod])
            S.barrier()

    def prenorm_T(self, xsrc, t0, ntiles, hT, bhT, col0, l, which, xring, nring, rring, key, halo=None):
        m = self.mod[l][which]
        gsc, sh = m["gsc" + key], m["sh" + key]
        for j in range(ntiles):
            xt, bxt = xring.next()
            if halo is None:
                self.load(xt[:], xsrc[t0 + j * 128:t0 + (j + 1) * 128, :], [bxt])
            else:
                self.op("pool", lambda e, xt=xt: e.memset(xt[:], 0.0), writes=[bxt])
                for hi_, tk in enumerate(halo):
                    if tk is not None:
                        self.load(xt[hi_:hi_ + 1, :], xsrc[tk:tk + 1, :], [bxt])
            xn, bxn = nring.next()
            rs, brs = rring.next()
            self.op("act", lambda e, xn=xn, xt=xt, rs=rs: e.activation(out=xn[:], in_=xt[:], func=AF.Square, accum_out=rs[:, 0:1]),
                    reads=[bxt], writes=[bxn, brs])
            self.op("act", lambda e, rs=rs: e.activation(out=rs[:, 1:2], in_=rs[:, 0:1], func=AF.Sqrt, scale=1.0 / D, bias=EPS),
                    reads=[brs], writes=[brs])
            self.op("dve", lambda e, rs=rs: e.reciprocal(out=rs[:, 2:3], in_=rs[:, 1:2]), reads=[brs], writes=[brs])
            self.op("dve", lambda e, xn=xn, xt=xt, rs=rs: e.tensor_scalar(out=xn[:], in0=xt[:], scalar1=rs[:, 2:3], scalar2=None,
                                                                     op0=ALU.mult), reads=[bxt, brs], writes=[bxn])
            for half in range(2):
                pt, bpt = self.PS[half]
                ptb = pt

                def em(e, half=half, ptb=ptb, xn=xn):
                    r = None
                    for k in range(8):
                        kc = half * 8 + k
                        r = e.transpose(out=ptb[:, k * 128:(k + 1) * 128], in_=xn[:, kc * 128:(kc + 1) * 128], identity=self.identb[:])
                    return r
                self.op("pe", em, reads=[bxn, self.bconst], writes=[bpt])
                for k in range(8):
                    kc = half * 8 + k
                    eng = "act"
                    dst = hT[:, kc, col0 + j * 128:col0 + (j + 1) * 128]
                    if eng == "act":
                        self.op("act", lambda e, dst=dst, ptb=ptb, k=k, kc=kc: e.activation(
                            out=dst, in_=ptb[:, k * 128:(k + 1) * 128], func=AF.Identity, scale=gsc[:, kc:kc + 1], bias=sh[:, kc:kc + 1]),
                            reads=[bpt, self.bmod], writes=[bhT[j][0]])
                    else:
                        self.op("dve", lambda e, dst=dst, ptb=ptb, k=k, kc=kc: e.tensor_scalar(
                            out=dst, in0=ptb[:, k * 128:(k + 1) * 128], scalar1=gsc[:, kc:kc + 1], scalar2=sh[:, kc:kc + 1],
                            op0=ALU.mult, op1=ALU.add), reads=[bpt, self.bmod], writes=[bhT[j][1]])

    def phaseA(self, l, xsrc, last):
        nc, S, I, Z = self.nc, self.S, self.I, self.Z
        TBA = 1024
        with contextlib.ExitStack() as st:
            hT, _ = self.sb(st, "hT", [128, KC, TBA], BF16)
            bh = [(Buf(), Buf()) for _ in range(TBA // 128)]
            xring = self.ring(st, "xa", [128, D], F32, 2)
            nring = self.ring(st, "xn", [128, D], BF16, 2)
            rring = self.ring(st, "rs", [128, 4], F32, 3)
            wring = self.ring(st, "win", [128, KC, 528], BF16, 3)
            wsrc = I["w_in"][l].rearrange("(k p) c -> p k c", p=128)
            cst, bcst = self.sb(st, "cstA", [128, 16 + 512 + 512 + 128 + 128], F32)
            gb = cst[:, 0:16]
            sgg = cst[:, 16:528]
            sgbb = cst[:, 528:1040]
            qg = cst[:, 1040:1168]
            kg = cst[:, 1168:1296]
            self.load(gb, I["ml_gate_b"][l].partition_broadcast(128), [bcst])
            self.load(sgg, I["sg_ln_g"][l].partition_broadcast(128), [bcst])
            self.load(sgbb, I["sg_ln_b"][l].partition_broadcast(128), [bcst])
            self.load(qg, I["at_qn_g"][l].partition_broadcast(128), [bcst])
            self.load(kg, I["at_kn_g"][l].partition_broadcast(128), [bcst])
            ef = self.ring(st, "ef", [128, 512], F32, 4)
            eb = self.ring(st, "eb", [128, 512], BF16, 4)
            sm = self.ring(st, "sm", [128, 16], F32, 4)
            csr = self.ring(st, "cs", [128, 128], F32, 2)
            psring = Ring(self.PS[2:8])
            cnt = [0]

            def copy_op(dst, src, reads, writes):
                cnt[0] += 1
                if cnt[0] % 2:
                    self.op("act", lambda e: e.activation(out=dst, in_=src, func=AF.Copy), reads=reads, writes=writes)
                else:
                    self.op("dve", lambda e: e.tensor_copy(out=dst, in_=src), reads=reads, writes=writes)

            blocks = [(0, 2, 1)] + [(TC + i * TBA, TBA // 128, 0) for i in range(TL // TBA)]
            for (t0, ntl, which) in blocks:
                n = ntl * 128
                if CUT < 0:
                    continue
                self.prenorm_T(xsrc, t0, ntl, hT, bh, 0, l, which, xring, nring, rring, "1")
                subs = [(s0, min(512, n - s0)) for s0 in range(0, n, 512)]

                def getpanel(c0, w):
                    wt, bwt = wring.next()
                    self.load(wt[:, :, 0:w], wsrc[:, :, c0:c0 + w], [bwt], q="pool")
                    return wt, bwt

                def fm_mm(wt, bwt, coff, t_lo, nn):
                    ps, bps = psring.next()

                    def em(e):
                        r = None
                        for kc in range(KC):
                            r = e.matmul(ps[:, 0:nn], lhsT=wt[:, kc, coff:coff + 128], rhs=hT[:, kc, t_lo:t_lo + nn],
                                         start=(kc == 0), stop=(kc == KC - 1))
                        return r
                    rb = [b for j in range(t_lo // 128, (t_lo + nn) // 128) for b in bh[j]]
                    self.op("pe", em, reads=[bwt] + rb, writes=[bps])
                    return ps, bps

                def tm_mm(wt, bwt, coff, ncols, j):
                    ps, bps = psring.next()

                    def em(e):
                        r = None
                        for kc in range(KC):
                            r = e.matmul(ps[:, 0:ncols], lhsT=hT[:, kc, j * 128:(j + 1) * 128], rhs=wt[:, kc, coff:coff + ncols],
                                         start=(kc == 0), stop=(kc == KC - 1))
                        return r
                    self.op("pe", em, reads=[bwt, bh[j][0], bh[j][1]], writes=[bps])
                    return ps, bps

                def fm_simple(c0, dst, func=None):
                    wt, bwt = getpanel(c0, 512)
                    for ch in range(4):
                        for (s0, nn) in subs:
                            ps, bps = fm_mm(wt, bwt, ch * 128, s0, nn)
                            o, bo = eb.next()
                            if func is None:
                                copy_op(o[:, 0:nn], ps[:, 0:nn], [bps], [bo])
                            else:
                                self.op("act", lambda e, o=o, ps=ps, nn=nn: e.activation(out=o[:, 0:nn], in_=ps[:, 0:nn], func=func),
                                        reads=[bps], writes=[bo])
                            self.store(dst[ch, :, t0 + s0:t0 + s0 + nn], o[:, 0:nn], [bo])
                    return wt, bwt

                def tm_simple(wt, bwt, coff, ncols, dst, func=None):
                    for j in range(ntl):
                        ps, bps = tm_mm(wt, bwt, coff, ncols, j)
                        o, bo = eb.next()
                        if func is None:
                            copy_op(o[:, 0:ncols], ps[:, 0:ncols], [bps], [bo])
                        else:
                            self.op("act", lambda e, o=o, ps=ps: e.activation(out=o[:, 0:ncols], in_=ps[:, 0:ncols], func=func),
                                    reads=[bps], writes=[bo])
                        self.store(dst[t0 + j * 128:t0 + (j + 1) * 128, :], o[:, 0:ncols], [bo])

                if CUT < 1:
                    continue
                fm_simple(0, Z["MQT"])
                if CUT < 2:
                    continue
                wt, bwt = fm_simple(512, Z["MKT"])
                tm_simple(wt, bwt, 0, 512, Z["MK"])
                if CUT < 3:
                    continue
                wt, bwt = getpanel(1024, 512)
                tm_simple(wt, bwt, 0, 512, Z["MV"])
                wt, bwt = getpanel(1536, 528)
                tm_simple(wt, bwt, 0, 512, Z["MO"], func=AF.Sigmoid)
                if CUT < 4:
                    continue
                for j in range(ntl):
                    ps, bps = tm_mm(wt, bwt, 512, 16, j)
                    o, bo = sm.next()
                    self.op("dve", lambda e, o=o, ps=ps: e.tensor_tensor(out=o[:], in0=ps[:, 0:16], in1=gb, op=ALU.add),
                            reads=[bps, bcst], writes=[bo])
                    self.store(Z["MG"][t0 + j * 128:t0 + (j + 1) * 128, :], o[:], [bo])
                if CUT < 5:
                    continue
                fm_simple(2064, Z["SU"], func=AF.Gelu_apprx_tanh)
                if CUT < 6:
                    continue
                wt, bwt = getpanel(2576, 512)
                for j in range(ntl):
                    ps, bps = tm_mm(wt, bwt, 0, 512, j)
                    g, bg = ef.next()
                    self.op("act", lambda e, g=g, ps=ps: e.activation(out=g[:], in_=ps[:], func=AF.Gelu_apprx_tanh), reads=[bps], writes=[bg])
                    s6, bs6 = sm.next()
                    self.op("dve", lambda e, s6=s6, g=g: e.bn_stats(out=s6[:, 0:6], in_=g[:]), reads=[bg], writes=[bs6])
                    self.op("dve", lambda e, s6=s6: e.bn_aggr(out=s6[:, 8:10], in_=s6[:, 0:6]), reads=[bs6], writes=[bs6])
                    self.op("act", lambda e, s6=s6: e.activation(out=s6[:, 10:11], in_=s6[:, 9:10], func=AF.Sqrt, scale=1.0, bias=EPS),
                            reads=[bs6], writes=[bs6])
                    self.op("dve", lambda e, s6=s6: e.reciprocal(out=s6[:, 11:12], in_=s6[:, 10:11]), reads=[bs6], writes=[bs6])
                    self.op("dve", lambda e, s6=s6, g=g: e.tensor_scalar(out=g[:], in0=g[:], scalar1=s6[:, 8:9], scalar2=s6[:, 11:12],
                                                                    op0=ALU.subtract, op1=ALU.mult), reads=[bg, bs6], writes=[bg])
                    self.op("pool", lambda e, g=g: e.tensor_tensor(out=g[:], in0=g[:], in1=sgg, op=ALU.mult), reads=[bg, bcst], writes=[bg])
                    o, bo = eb.next()
                    self.op("pool", lambda e, g=g, o=o: e.tensor_tensor(out=o[:], in0=g[:], in1=sgbb, op=ALU.add), reads=[bg, bcst], writes=[bo])
                    self.store(Z["SV"][t0 + j * 128:t0 + (j + 1) * 128, :], o[:], [bo])
                if CUT < 7:
                    continue
                wa, bwa = getpanel(3088, 512)
                wg, bwg = getpanel(3600, 512)
                for ch in range(4):
                    for (s0, nn) in subs:
                        pa, bpa = fm_mm(wa, bwa, ch * 128, s0, nn)
                        pg, bpg = fm_mm(wg, bwg, ch * 128, s0, nn)
                        g, bg = ef.next()
                        self.op("act", lambda e, g=g, pg=pg, nn=nn: e.activation(out=g[:, 0:nn], in_=pg[:, 0:nn], func=AF.Sigmoid),
                                reads=[bpg], writes=[bg])
                        o, bo = ef.next()
                        self.op("dve", lambda e, o=o, pa=pa, g=g, nn=nn: e.tensor_tensor(out=o[:, 0:nn], in0=pa[:, 0:nn], in1=g[:, 0:nn], op=ALU.mult),
                                reads=[bpa, bg], writes=[bo])
                        self.store(Z["CY"][ch, :, t0 + s0:t0 + s0 + nn], o[:, 0:nn], [bo])
                def qk_path(wt, bwt, coff, nh, gain, dst):
                    for j in range(ntl):
                        ps, bps = tm_mm(wt, bwt, coff, nh * 128, j)
                        w_ = nh * 128
                        g, bg = ef.next()
                        self.op("act", lambda e, g=g, ps=ps: e.activation(out=g[:, 0:w_], in_=ps[:, 0:w_], func=AF.Square), reads=[bps], writes=[bg])
                        s6, bs6 = sm.next()
                        self.op("dve", lambda e, s6=s6, g=g: e.tensor_reduce(out=s6[:, 0:nh], in_=g[:, 0:w_].rearrange("p (h d) -> p h d", h=nh),
                                                                        axis=AX.X, op=ALU.add), reads=[bg], writes=[bs6])
                        self.op("act", lambda e, s6=s6: e.activation(out=s6[:, 4:4 + nh], in_=s6[:, 0:nh], func=AF.Sqrt, scale=1.0 / 128, bias=EPS),
                                reads=[bs6], writes=[bs6])
                        self.op("dve", lambda e, s6=s6: e.reciprocal(out=s6[:, 8:8 + nh], in_=s6[:, 4:4 + nh]), reads=[bs6], writes=[bs6])
                        q3 = g[:, 0:w_].rearrange("p (h d) -> p h d", h=nh)
                        self.op("dve", lambda e, q3=q3, ps=ps, s6=s6: e.tensor_tensor(
                            out=q3, in0=ps[:, 0:w_].rearrange("p (h d) -> p h d", h=nh),
                            in1=s6[:, 8:8 + nh].unsqueeze(2).to_broadcast([128, nh, 128]), op=ALU.mult), reads=[bps, bs6], writes=[bg])
                        qr, bqr = eb.next()
                        qr3 = qr[:, 0:w_].rearrange("p (h d) -> p h d", h=nh)
                        gn3 = gain.unsqueeze(1).to_broadcast([128, nh, 128])
                        if which == 0:
                            self.op("pool", lambda e, q3=q3: e.tensor_tensor(out=q3, in0=q3, in1=gn3, op=ALU.mult), reads=[bg, bcst], writes=[bg])
                            cs, bcs = csr.next()
                            tl0 = t0 - TC + j * 128
                            self.load(cs[:], I["rope"][tl0:tl0 + 128, :], [bcs])
                            C3 = cs[:, 0:64].unsqueeze(1).to_broadcast([128, nh, 64])
                            S3 = cs[:, 64:128].unsqueeze(1).to_broadcast([128, nh, 64])
                            t, bt = ef.next()
                            t3 = t[:, 0:w_].rearrange("p (h d) -> p h d", h=nh)
                            x1, x2 = q3[:, :, 0:64], q3[:, :, 64:128]
                            self.op("dve", lambda e: e.tensor_tensor(out=t3[:, :, 0:64], in0=x1, in1=C3, op=ALU.mult), reads=[bg, bcs], writes=[bt])
                            self.op("pool", lambda e: e.tensor_tensor(out=t3[:, :, 64:128], in0=x2, in1=S3, op=ALU.mult), reads=[bg, bcs], writes=[bt])
                            self.op("dve", lambda e: e.tensor_tensor(out=qr3[:, :, 0:64], in0=t3[:, :, 0:64], in1=t3[:, :, 64:128], op=ALU.subtract),
                                    reads=[bt], writes=[bqr])
                            t_, bt_ = ef.next()
                            u3 = t_[:, 0:w_].rearrange("p (h d) -> p h d", h=nh)
                            self.op("dve", lambda e: e.tensor_tensor(out=u3[:, :, 0:64], in0=x2, in1=C3, op=ALU.mult), reads=[bg, bcs], writes=[bt_])
                            self.op("pool", lambda e: e.tensor_tensor(out=u3[:, :, 64:128], in0=x1, in1=S3, op=ALU.mult), reads=[bg, bcs], writes=[bt_])
                            self.op("dve", lambda e: e.tensor_tensor(out=qr3[:, :, 64:128], in0=u3[:, :, 0:64], in1=u3[:, :, 64:128], op=ALU.add),
                                    reads=[bt_], writes=[bqr])
                        else:
                            self.op("dve", lambda e, q3=q3: e.tensor_tensor(out=qr3, in0=q3, in1=gn3, op=ALU.mult), reads=[bg, bcst], writes=[bqr])
                        pt, bpt = self.PS[j % 2]
                        ptb = pt

                        def em(e, qr=qr, ptb=ptb):
                            r = None
                            for h in range(nh):
                                r = e.transpose(out=ptb[:, h * 128:(h + 1) * 128], in_=qr[:, h * 128:(h + 1) * 128], identity=self.identb[:])
                            return r
                        self.op("pe", em, reads=[bqr, self.bconst], writes=[bpt])
                        o, bo = eb.next()
                        self.op("act", lambda e, o=o, ptb=ptb: e.activation(out=o[:, 0:w_], in_=ptb[:, 0:w_], func=AF.Copy), reads=[bpt], writes=[bo])
                        self.store(dst[:, :, t0 + j * 128:t0 + (j + 1) * 128].rearrange("h d t -> d h t"),
                                   o[:, 0:w_].rearrange("p (h t) -> p h t", h=nh), [bo])

                if CUT < 8:
                    continue
                wt, bwt = getpanel(4112, 512)
                if not (last and which == 1):
                    qk_path(wt, bwt, 0, 4, qg, Z["AQT"])
                wt, bwt = getpanel(4624, 512)
                qk_path(wt, bwt, 0, 2, kg, Z["AKT"])
                tm_simple(wt, bwt, 256, 256, Z["AV"])
            S.barrier()

    def phaseB(self, l, last):
        nc, S, I, Z = self.nc, self.S, self.I, self.Z
        with contextlib.ExitStack() as st:
            ngb, bngb = self.sb(st, "ngb", [128, 512], F32)
            self.load(ngb[:], I["ml_norm_g"][l].partition_broadcast(128), [bngb])
            gr = self.ring(st, "gates", [128, 16], F32, 3)
            sc = self.ring(st, "mlsc", [128, 32], F32, 3)
            kTr = self.ring(st, "kT4", [128, 4, 128], BF16, 3)
            qTr = self.ring(st, "qT4", [128, 4, 128], BF16, 3)
            kkr = self.ring(st, "kk", [128, 512], BF16, 3)
            ver = self.ring(st, "vext", [128, 4, 130], BF16, 3)
            vpr = self.ring(st, "vp", [128, 4, 130], BF16, 3)
            for (ve, bve) in ver.tiles:
                self.op("pool", lambda e, ve=ve: e.memset(ve[:], 1.0), writes=[bve])
            sTr = self.ring(st, "sT", [128, 128], BF16, 4)
            hbr = self.ring(st, "hbuf", [128, 512], F32, 3)
            hfr = self.ring(st, "hfl", [128, 512], F32, 2)
            mor = self.ring(st, "mo", [128, 512], BF16, 2)
            yar = self.ring(st, "ya", [128, 512], BF16, 2)
            yor = self.ring(st, "yo", [128, 512], BF16, 2)
            smr = self.ring(st, "smB", [128, 16], F32, 6)
            Cst = [[self.sb(st, "Cst", [128, 129], F32) for h in range(4)] for d in range(2)]
            Cb = [[self.sb(st, "Cb", [128, 129], BF16) for h in range(4)] for d in range(2)]
            psA = Ring(self.PS[2:4])
            psN = Ring(self.PS[4:6])
            psC = Ring(self.PS[6:8])
            LNS = float(np.log(128.0 ** -0.5))
            for d in range(2):
                for h in range(4):
                    c_, bc_ = Cst[d][h]
                    self.op("pool", lambda e, c_=c_: e.memset(c_[:], 0.0), writes=[bc_])
                    cb_, bcb_ = Cb[d][h]
                    self.op("pool", lambda e, cb_=cb_: e.memset(cb_[:], 0.0), writes=[bcb_])
                order = list(range(NT)) if d == 0 else [1, 0] + list(range(NT - 1, 1, -1))
                mk = self.mkF if d == 0 else self.mkB
                for c in order:
                    need_h = not (last and c < 2)
                    tsl = slice(c * 128, (c + 1) * 128)
                    g, bg = gr.next()
                    self.load(g[:], Z["MG"][tsl, :], [bg])
                    s_, bs_ = sc.next()
                    self.op("act", lambda e: e.activation(out=s_[:, 0:4], in_=g[:, d * 8 + 4:d * 8 + 8], func=AF.Exp, scale=-1.0), reads=[bg], writes=[bs_])
                    self.op("act", lambda e: e.activation(out=s_[:, 0:4], in_=s_[:, 0:4], func=AF.Ln, bias=1.0), reads=[bs_], writes=[bs_])
                    pc, bpc = psC.next()

                    def em(e):
                        e.matmul(pc[:, 0:4], lhsT=mk[:], rhs=s_[:, 0:4], start=True, stop=True)
                        return e.matmul(pc[:, 8:12], lhsT=self.onesf[:], rhs=s_[:, 0:4], start=True, stop=True)
                    self.op("pe", em, reads=[bs_, self.bconst], writes=[bpc])
                    self.op("dve", lambda e: e.tensor_tensor(out=s_[:, 4:8], in0=pc[:, 0:4], in1=g[:, d * 8:d * 8 + 4], op=ALU.add), reads=[bpc, bg], writes=[bs_])
                    self.op("dve", lambda e: e.tensor_scalar(out=s_[:, 4:8], in0=s_[:, 4:8], scalar1=LNS, scalar2=None, op0=ALU.add), reads=[bs_], writes=[bs_])
                    self.op("act", lambda e: e.activation(out=s_[:, 4:8], in_=s_[:, 4:8], func=AF.Exp), reads=[bs_], writes=[bs_])
                    self.op("act", lambda e: e.activation(out=s_[:, 8:12], in_=pc[:, 0:4], func=AF.Exp), reads=[bpc], writes=[bs_])
                    self.op("act", lambda e: e.activation(out=s_[:, 12:16], in_=pc[:, 8:12], func=AF.Exp, scale=-1.0), reads=[bpc], writes=[bs_])
                    kT, bkT = kTr.next()
                    self.load(kT[:], Z["MKT"][:, :, tsl].rearrange("h d t -> d h t"), [bkT])
                    kk, bkk = kkr.next()
                    self.load(kk[:], Z["MK"][tsl, :], [bkk])
                    ve, bve = ver.next()
                    self.load(ve[:, :, 0:128], Z["MV"][tsl, :].rearrange("t (h e) -> t h e", h=4), [bve])
                    vp, bvp = vpr.next()
                    if need_h:
                        qT, bqT = qTr.next()
                        self.load(qT[:], Z["MQT"][:, :, tsl].rearrange("h d t -> d h t"), [bqT])
                        hb, bhb = hbr.next()
                    for h in range(4):
                        c_, bc_ = Cst[d][h]
                        cb_, bcb_ = Cb[d][h]
                        self.op("dve", lambda e: e.tensor_scalar(out=vp[:, h, 0:129], in0=ve[:, h, 0:129], scalar1=s_[:, 4 + h:5 + h], scalar2=None, op0=ALU.mult),
                                reads=[bve, bs_], writes=[bvp])
                        if need_h:
                            pa, bpa = psA.next()
                            self.op("pe", lambda e: e.matmul(pa[:, 0:128], lhsT=kT[:, h, :], rhs=qT[:, h, :], start=True, stop=True), reads=[bkT, bqT], writes=[bpa])
                            sT, bsT = sTr.next()
                            self.op("dve", lambda e: e.scalar_tensor_tensor(out=sT[:], in0=pa[:, 0:128], scalar=s_[:, 4 + h:5 + h], in1=mk[:], op0=ALU.mult, op1=ALU.mult),
                                    reads=[bpa, bs_, self.bconst], writes=[bsT])
                            pn, bpn = psN.next()

                            def em(e):
                                e.matmul(pn[:, 0:129], lhsT=qT[:, h, :], rhs=cb_[:], start=True, stop=False)
                                return e.matmul(pn[:, 0:129], lhsT=sT[:], rhs=ve[:, h, 0:129], start=False, stop=True)
                            self.op("pe", em, reads=[bqT, bcb_, bsT, bve], writes=[bpn])
                            r_, br_ = smr.next()
                            self.op("act", lambda e: e.activation(out=r_[:, 0:1], in_=pn[:, 128:129], func=AF.Abs), reads=[bpn], writes=[br_])
                            self.op("dve", lambda e: e.tensor_tensor(out=r_[:, 0:1], in0=r_[:, 0:1], in1=s_[:, 8 + h:9 + h], op=ALU.max),
                                    reads=[br_, bs_], writes=[br_])
                            self.op("dve", lambda e: e.reciprocal(out=r_[:, 1:2], in_=r_[:, 0:1]), reads=[br_], writes=[br_])
                            self.op("act", lambda e: e.activation(out=hb[:, h * 128:(h + 1) * 128], in_=pn[:, 0:128], func=AF.Identity, scale=r_[:, 1:2]),
                                    reads=[bpn, br_], writes=[bhb])
                        pc2, bpc2 = psC.next()
                        self.op("pe", lambda e: e.matmul(pc2[:, 0:129], lhsT=kk[:, h * 128:(h + 1) * 128], rhs=vp[:, h, 0:129], start=True, stop=True), reads=[bkk, bvp], writes=[bpc2])
                        self.op("dve", lambda e: e.tensor_tensor(out=c_[:], in0=pc2[:, 0:129], in1=c_[:], op=ALU.add), reads=[bpc2, bc_], writes=[bc_])
                        self.op("act", lambda e: e.activation(out=c_[:], in_=c_[:], func=AF.Identity, scale=s_[:, 12 + h:13 + h]), reads=[bc_, bs_], writes=[bc_])
                        self.op("pool", lambda e: e.tensor_copy(out=cb_[:], in_=c_[:]), reads=[bc_], writes=[bcb_])
                    if not need_h:
                        continue
                    if d == 0:
                        self.store(Z["HF"][tsl, :], hb[:], [bhb])
                        continue
                    hf, bhf = hfr.next()
                    self.load(hf[:], Z["HF"][tsl, :], [bhf])
                    mo, bmo = mor.next()
                    self.load(mo[:], Z["MO"][tsl, :], [bmo])
                    self.op("dve", lambda e: e.tensor_tensor(out=hb[:], in0=hb[:], in1=hf[:], op=ALU.add), reads=[bhb, bhf], writes=[bhb])
                    ya, bya = yar.next()
                    for h in range(4):
                        hs = slice(h * 128, (h + 1) * 128)
                        r_, br_ = smr.next()
                        self.op("dve", lambda e: e.bn_stats(out=r_[:, 0:6], in_=hb[:, hs]), reads=[bhb], writes=[br_])
                        self.op("dve", lambda e: e.bn_aggr(out=r_[:, 8:10], in_=r_[:, 0:6]), reads=[br_], writes=[br_])
                        self.op("act", lambda e: e.activation(out=r_[:, 10:11], in_=r_[:, 9:10], func=AF.Sqrt, scale=1.0, bias=EPS), reads=[br_], writes=[br_])
                        self.op("dve", lambda e: e.reciprocal(out=r_[:, 11:12], in_=r_[:, 10:11]), reads=[br_], writes=[br_])
                        self.op("dve", lambda e: e.tensor_scalar(out=hb[:, hs], in0=hb[:, hs], scalar1=r_[:, 8:9], scalar2=r_[:, 11:12], op0=ALU.subtract, op1=ALU.mult),
                                reads=[bhb, br_], writes=[bhb])
                    self.op("pool", lambda e: e.tensor_tensor(out=hb[:], in0=hb[:], in1=ngb[:], op=ALU.mult), reads=[bhb, bngb], writes=[bhb])
                    self.op("dve", lambda e: e.tensor_tensor(out=ya[:], in0=hb[:], in1=mo[:], op=ALU.mult), reads=[bhb, bmo], writes=[bya])
                    pt, bpt = self.PS[c % 2]

                    def em(e):
                        r = None
                        for h in range(4):
                            r = e.transpose(out=pt[:, h * 128:(h + 1) * 128], in_=ya[:, h * 128:(h + 1) * 128], identity=self.identb[:])
                        return r
                    self.op("pe", em, reads=[bya, self.bconst], writes=[bpt])
                    yo, byo = yor.next()
                    self.op("act", lambda e: e.activation(out=yo[:], in_=pt[:, 0:512], func=AF.Copy), reads=[bpt], writes=[byo])
                    self.store(Z["YT"][0:4, :, tsl].rearrange("h d t -> d h t"), yo[:].rearrange("p (h t) -> p h t", h=4), [byo])
            S.barrier()

    def phaseC(self, l, last):
        nc, S, I, Z = self.nc, self.S, self.I, self.Z
        with contextlib.ExitStack() as st:
            wsT, bws = self.sb(st, "wsT", [128, 4, 128], BF16)
            wst, bwst = self.sb(st, "wstg", [128, 128], F32)
            for g in range(4):
                self.load(wst[:], I["sg_w"][l, g], [bwst])
                ps, bps = self.PS[7]
                self.op("pe", lambda e: e.transpose(out=ps[:, 0:128], in_=wst[:], identity=self.ident[:]), reads=[bwst, self.bconst], writes=[bps])
                self.op("dve", lambda e: e.tensor_copy(out=wsT[:, g, :], in_=ps[:, 0:128]), reads=[bps], writes=[bws])
            bsf, bbsf = self.sb(st, "bsf", [1, 512], F32)
            self.load(bsf[:], I["sg_b"][l:l + 1].rearrange("o g t -> o (g t)"), [bbsf])
            bsb, bbsb = self.sb(st, "bsb", [1, 512], BF16)
            self.op("dve", lambda e: e.tensor_copy(out=bsb[:], in_=bsf[:]), reads=[bbsf], writes=[bbsb])
            svr = self.ring(st, "sv", [128, 512], BF16, 3)
            sur = self.ring(st, "su", [128, 4, 128], BF16, 3)
            ybr = self.ring(st, "yb", [128, 4, 128], BF16, 3)
            psr = Ring(self.PS[2:7])
            for c in range(2 if last else 0, NT):
                tsl = slice(c * 128, (c + 1) * 128)
                sv, bsv = svr.next()
                self.load(sv[:], Z["SV"][tsl, :], [bsv])
                su, bsu = sur.next()
                self.load(su[:], Z["SU"][:, :, tsl].rearrange("g c t -> c g t"), [bsu])
                yb, byb = ybr.next()
                for g in range(4):
                    ps, bps = psr.next()

                    def em(e):
                        e.matmul(ps[:, 0:128], lhsT=sv[:, g * 128:(g + 1) * 128], rhs=wsT[:, g, :], start=True, stop=False)
                        return e.matmul(ps[:, 0:128], lhsT=self.onesb[0:1, :], rhs=bsb[0:1, g * 128:(g + 1) * 128], start=False, stop=True)
                    self.op("pe", em, reads=[bsv, bws, bbsb, self.bconst], writes=[bps])
                    self.op("dve", lambda e: e.tensor_tensor(out=yb[:, g, :], in0=ps[:, 0:128], in1=su[:, g, :], op=ALU.mult), reads=[bps, bsu], writes=[byb])
                self.store(Z["YT"][4:8, :, tsl].rearrange("g c t -> c g t"), yb[:], [byb])
            S.barrier()

    def phaseD(self, l, last):
        nc, S, I, Z = self.nc, self.S, self.I, self.Z
        with contextlib.ExitStack() as st:
            wfm, bwfm = self.sb(st, "cvw", [128, 124 + 12], F32)
            self.load_fm(st, wfm[:, 0:124], bwfm, I["cv_w"][l].rearrange("k (c p) -> (k c) p", p=128), 124)
            self.load_fm(st, wfm[:, 124:128], bwfm, I["cv_b"][l].rearrange("(c p) -> c p", p=128), 4)
            self.load_fm(st, wfm[:, 128:132], bwfm, I["cv_ln_g"][l].rearrange("(c p) -> c p", p=128), 4)
            self.load_fm(st, wfm[:, 132:136], bwfm, I["cv_ln_b"][l].rearrange("(c p) -> c p", p=128), 4)
            dg, bdg = self.sb(st, "diag", [128, 124, 128], F32)
            for r in range(124):
                eng = "dve"
                self.op(eng, lambda e: e.tensor_scalar(out=dg[:, r, :], in0=self.ident[:], scalar1=wfm[:, r:r + 1], scalar2=None, op0=ALU.mult),
                        reads=[bwfm, self.bconst], writes=[bdg])
            ybr = [self.ring(st, "cyb", [128, 542], F32, 2) for ch in range(4)]
            y2r = [self.ring(st, "y2", [128, 512], F32, 2) for ch in range(4)]
            sqr = self.ring(st, "sq", [128, 512], F32, 2)
            str_ = self.ring(st, "stt", [128, 512], F32, 6)
            yor = self.ring(st, "cyo", [128, 512], BF16, 3)
            seqs = ([] if last else [(0, TC)]) + [(TC, T)]
            for (s0, s1) in seqs:
                for t0 in range(s0, s1, 512):
                    n = min(512, s1 - t0)
                    lo = max(s0, t0 - 15)
                    hi = min(s1, t0 + n + 15)
                    y2s = []
                    for ch in range(4):
                        yb, byb = ybr[ch].next()
                        if lo > t0 - 15:
                            self.op("pool", lambda e: e.memset(yb[:, 0:15], 0.0), writes=[byb])
                        if hi < t0 + n + 15:
                            self.op("pool", lambda e: e.memset(yb[:, n + 15:n + 30], 0.0), writes=[byb])
                        self.load(yb[:, lo - (t0 - 15):hi - (t0 - 15)], Z["CY"][ch, :, lo:hi], [byb])
                        ps, bps = self.PS[2 + ch]

                        def em(e):
                            r = None
                            for k in range(31):
                                r = e.matmul(ps[:, 0:n], lhsT=dg[:, k * 4 + ch, :], rhs=yb[:, k:k + n], start=(k == 0), stop=(k == 30))
                            return r
                        self.op("pe", em, reads=[bdg, byb], writes=[bps])
                        y2, by2 = y2r[ch].next()
                        self.op("act", lambda e: e.activation(out=y2[:, 0:n], in_=ps[:, 0:n], func=AF.Identity, bias=wfm[:, 124 + ch:125 + ch], scale=1.0), reads=[bps, bwfm], writes=[by2])
                        y2s.append((y2, by2))
                    p1, bp1 = self.PS[6]
                    p2, bp2 = self.PS[7]

                    def em(e):
                        r = None
                        for ch in range(4):
                            r = e.matmul(p1[:, 0:n], lhsT=self.onesf[:], rhs=y2s[ch][0][:, 0:n], start=(ch == 0), stop=(ch == 3))
                        return r
                    self.op("pe", em, reads=[b for (_, b) in y2s] + [self.bconst], writes=[bp1])
                    sqs = []
                    for ch in range(4):
                        sq, bsq = sqr.next()
                        self.op("act", lambda e: e.activation(out=sq[:, 0:n], in_=y2s[ch][0][:, 0:n], func=AF.Square), reads=[y2s[ch][1]], writes=[bsq])
                        self.op("pe", lambda e: e.matmul(p2[:, 0:n], lhsT=self.onesf[:], rhs=sq[:, 0:n], start=(ch == 0), stop=(ch == 3)), reads=[bsq, self.bconst], writes=[bp2])
                    mean, bmean = str_.next()
                    self.op("dve", lambda e: e.tensor_scalar(out=mean[:, 0:n], in0=p1[:, 0:n], scalar1=1.0 / 512, scalar2=None, op0=ALU.mult), reads=[bp1], writes=[bmean])
                    m2, bm2 = str_.next()
                    self.op("pool", lambda e: e.tensor_tensor(out=m2[:, 0:n], in0=mean[:, 0:n], in1=mean[:, 0:n], op=ALU.mult), reads=[bmean], writes=[bm2])
                    var, bvar = str_.next()
                    self.op("dve", lambda e: e.scalar_tensor_tensor(out=var[:, 0:n], in0=p2[:, 0:n], scalar=1.0 / 512, in1=m2[:, 0:n], op0=ALU.mult, op1=ALU.subtract),
                            reads=[bp2, bm2], writes=[bvar])
                    self.op("act", lambda e: e.activation(out=var[:, 0:n], in_=var[:, 0:n], func=AF.Sqrt, scale=1.0, bias=EPS), reads=[bvar], writes=[bvar])
                    self.op("dve", lambda e: e.reciprocal(out=var[:, 0:n], in_=var[:, 0:n]), reads=[bvar], writes=[bvar])
                    for ch in range(4):
                        y2, by2 = y2s[ch]
                        self.op("dve", lambda e: e.tensor_tensor(out=y2[:, 0:n], in0=y2[:, 0:n], in1=mean[:, 0:n], op=ALU.subtract), reads=[by2, bmean], writes=[by2])
                        self.op("pool", lambda e: e.tensor_tensor(out=y2[:, 0:n], in0=y2[:, 0:n], in1=var[:, 0:n], op=ALU.mult), reads=[by2, bvar], writes=[by2])
                        yo, byo = yor.next()
                        self.op("act", lambda e: e.activation(out=yo[:, 0:n], in_=y2[:, 0:n], func=AF.Silu, scale=wfm[:, 128 + ch:129 + ch], bias=wfm[:, 132 + ch:133 + ch]),
                                reads=[by2, bwfm], writes=[byo])
                        self.store(Z["YT"][8 + ch, :, t0:t0 + n], yo[:, 0:n], [byo])
            S.barrier()

    def phaseE(self, l, last):
        nc, S, I, Z = self.nc, self.S, self.I, self.Z
        with contextlib.ExitStack() as st:
            akt, bakt = self.sb(st, "akt", [128, 2, T], BF16)
            self.load(akt[:], Z["AKT"].rearrange("h d t -> d h t"), [bakt])
            av, bav = self.sb(st, "av", [128, NT, 256], BF16)
            self.load(av[:], Z["AV"].rearrange("(n p) c -> p n c", p=128), [bav])
            gq, bgq = self.sb(st, "gq", [128, 260], F32)
            self.load(gq[:, 0:128], I["at_qn_g"][l].partition_broadcast(128), [bgq])
            self.load(gq[:, 128:256], I["at_kn_g"][l].partition_broadcast(128), [bgq])
            self.op("dve", lambda e: e.tensor_reduce(out=gq[:, 256:257], in_=gq[:, 0:128], axis=AX.X, op=ALU.max, apply_absolute_value=True), reads=[bgq], writes=[bgq])
            self.op("dve", lambda e: e.tensor_reduce(out=gq[:, 257:258], in_=gq[:, 128:256], axis=AX.X, op=ALU.max, apply_absolute_value=True), reads=[bgq], writes=[bgq])
            self.op("dve", lambda e: e.scalar_tensor_tensor(out=gq[:, 258:259], in0=gq[:, 256:257], scalar=-float(128.0 ** 0.5), in1=gq[:, 257:258], op0=ALU.mult, op1=ALU.mult),
                    reads=[bgq], writes=[bgq])
            negC = gq[:, 258:259]
            qr = self.ring(st, "aq", [128, 512], BF16, 2)
            pr = self.ring(st, "ap", [128, 512], BF16, 4)
            rsr = self.ring(st, "ars", [128, 512], F32, 2)
            yor = self.ring(st, "ayo", [128, 512], BF16, 2)
            psS = Ring(self.PS[2:6])
            acc = Ring([(self.PS[6], self.PS[7])])
            SCL = float(128.0 ** -0.5)
            jobs = []
            if not last:
                for h in range(4):
                    jobs.append((h, 0, 256, [0, 1]))
            for h in range(4):
                for tc in range(TL // 512):
                    jobs.append((h, TC + tc * 512, 512, list(range(NT))))
            for (h, t0, n, stiles) in jobs:
                kvh = h // 2
                q, bq = qr.next()
                self.load(q[:, 0:n], Z["AQT"][h, :, t0:t0 + n], [bq])
                (po, bpo), (psm, bpsm) = self.PS[6], self.PS[7]
                for i, s in enumerate(stiles):
                    ps, bps = psS.next()
                    self.op("pe", lambda e: e.matmul(ps[:, 0:n], lhsT=akt[:, kvh, s * 128:(s + 1) * 128], rhs=q[:, 0:n], start=True, stop=True), reads=[bakt, bq], writes=[bps])
                    p, bp = pr.next()
                    self.op("act", lambda e: e.activation(out=p[:, 0:n], in_=ps[:, 0:n], func=AF.Exp, scale=SCL, bias=negC), reads=[bps, bgq], writes=[bp])
                    first, lastk = (i == 0), (i == len(stiles) - 1)

                    def em(e):
                        e.matmul(po[:, 0:n], lhsT=av[:, s, kvh * 128:(kvh + 1) * 128], rhs=p[:, 0:n], start=first, stop=lastk)
                        return e.matmul(psm[:, 0:n], lhsT=self.onesb[:], rhs=p[:, 0:n], start=first, stop=lastk)
                    self.op("pe", em, reads=[bav, bp, self.bconst], writes=[bpo, bpsm])
                rs, brs = rsr.next()
                self.op("dve", lambda e: e.reciprocal(out=rs[:, 0:n], in_=psm[:, 0:n]), reads=[bpsm], writes=[brs])
                yo, byo = yor.next()
                self.op("dve", lambda e: e.tensor_tensor(out=yo[:, 0:n], in0=po[:, 0:n], in1=rs[:, 0:n], op=ALU.mult), reads=[bpo, brs], writes=[byo])
                self.store(Z["YT"][12 + h, :, t0:t0 + n], yo[:, 0:n], [byo])
            S.barrier()

    def phaseF(self, l, xsrc, last):
        nc, S, I, Z = self.nc, self.S, self.I, self.Z
        with contextlib.ExitStack() as st:
            wo, bwo = self.sb(st, "wo", [128, KC, D], BF16)
            wsrc = I["w_out"][l].rearrange("(k p) c -> p k c", p=128)
            for q in range(4):
                self.load(wo[:, :, q * 512:(q + 1) * 512], wsrc[:, :, q * 512:(q + 1) * 512], [bwo], q="pool")
            G, bG = self.sb(st, "G1", [128, 2, D], F32)
            for w in range(2):
                self.load(G[:, w, :], Z["GV"][l, w, 0].partition_broadcast(128), [bG])
            yr = self.ring(st, "yT", [128, KC, 128], BF16, 3)
            xr = self.ring(st, "xF", [128, D], F32, 3)
            orr = self.ring(st, "oF", [128, D], F32, 2)
            jr = self.ring(st, "jF", [128, 512], BF16, 2)
            smr = self.ring(st, "smF", [128, 8], F32, 3)
            for c in range(2 if last else 0, NT):
                which = 1 if c < 2 else 0
                tsl = slice(c * 128, (c + 1) * 128)
                y, by = yr.next()
                self.load(y[:], Z["YT"][:, :, tsl].rearrange("k p t -> p k t"), [by])
                x, bx = xr.next()
                self.load(x[:], xsrc[tsl, :], [bx])
                sm, bsm = smr.next()
                for dc in range(4):
                    ps, bps = self.PS[2 + dc]

                    def em(e):
                        r = None
                        for kc in range(KC):
                            r = e.matmul(ps[:], lhsT=y[:, kc, :], rhs=wo[:, kc, dc * 512:(dc + 1) * 512], start=(kc == 0), stop=(kc == KC - 1))
                        return r
                    self.op("pe", em, reads=[by, bwo], writes=[bps])
                    j_, bj_ = jr.next()
                    self.op("act", lambda e: e.activation(out=j_[:], in_=ps[:], func=AF.Square, accum_out=sm[:, dc:dc + 1]), reads=[bps], writes=[bj_, bsm])
                self.op("dve", lambda e: e.tensor_reduce(out=sm[:, 4:5], in_=sm[:, 0:4], axis=AX.X, op=ALU.add), reads=[bsm], writes=[bsm])
                self.op("act", lambda e: e.activation(out=sm[:, 5:6], in_=sm[:, 4:5], func=AF.Sqrt, scale=1.0 / D, bias=EPS), reads=[bsm], writes=[bsm])
                self.op("dve", lambda e: e.reciprocal(out=sm[:, 6:7], in_=sm[:, 5:6]), reads=[bsm], writes=[bsm])
                o, bo = orr.next()
                for dc in range(4):
                    ps, bps = self.PS[2 + dc]
                    dsl = slice(dc * 512, (dc + 1) * 512)
                    self.op("dve", lambda e: e.scalar_tensor_tensor(out=o[:, dsl], in0=ps[:], scalar=sm[:, 6:7], in1=G[:, which, dsl], op0=ALU.mult, op1=ALU.mult),
                            reads=[bps, bsm, bG], writes=[bo])
                self.op("pool", lambda e: e.tensor_tensor(out=o[:], in0=o[:], in1=x[:], op=ALU.add), reads=[bo, bx], writes=[bo])
                self.store(Z["XA"][tsl, :], o[:], [bo])
            S.barrier()

    def phaseG(self, l, last):
        nc, S, I, Z = self.nc, self.S, self.I, self.Z
        TBG = 512
        with contextlib.ExitStack() as st:
            cw, bcw = self.sb(st, "fcw", [128, 132 + 44], F32)
            src = I["ffn_cv_w"][l].rearrange("k (f p) -> (k f) p", p=128)
            self.load_fm(st, cw[:, 0:128], bcw, src[0:128, :], 128)
            self.load_fm(st, cw[:, 128:132], bcw, src[128:132, :], 4)
            self.load_fm(st, cw[:, 132:176], bcw, I["ffn_cv_b"][l].rearrange("(f p) -> f p", p=128), 44)
            G, bG = self.sb(st, "G2", [128, 2, D], F32)
            for w in range(2):
                self.load(G[:, w, :], Z["GV"][l, w, 1].partition_broadcast(128), [bG])
            hT, _ = self.sb(st, "hT2", [128, KC, TBG + 128], BF16)
            bh = [(Buf(), Buf()) for _ in range(TBG // 128 + 1)]
            aT, _ = self.sb(st, "aT", [128, FC, TBG], BF16)
            ba = [Buf() for _ in range(FC)]
            xring = self.ring(st, "xg", [128, D], F32, 3)
            nring = self.ring(st, "xng", [128, D], BF16, 2)
            rring = self.ring(st, "rsg", [128, 4], F32, 3)
            wur = self.ring(st, "wu", [128, KC, 256], BF16, 2)
            wdr = self.ring(st, "wd", [128, FC, 256], BF16, 2)
            gbr = self.ring(st, "gbuf", [128, TBG + 2], F32, 2)
            tr_ = self.ring(st, "tg", [128, TBG], F32, 3)
            er = self.ring(st, "eg", [128, 256], F32, 3)
            jr = self.ring(st, "jg", [128, 256], BF16, 2)
            ssq, bssq = self.sb(st, "ssq", [128, 32], F32)
            wusrc = I["w_up"][l].rearrange("(k p) c -> p k c", p=128)
            wdsrc = I["w_down"][l].rearrange("(f p) d -> p f d", p=128)
            psg = Ring(self.PS[2:6])
            seqs = ([] if last else [(0, TC, 1)]) + [(TC, T, 0)]
            for (s0, s1, which) in seqs:
                for t0 in range(s0, s1, TBG):
                    n = min(TBG, s1 - t0)
                    ntl = n // 128
                    GC = int(os.environ.get("GCUT", "9"))
                    if GC < 2:
                        continue
                    self.prenorm_T(Z["XA"], t0, ntl, hT, bh, 0, l, which, xring, nring, rring, "2")
                    self._halo = (t0 - 1 if t0 - 1 >= s0 else None, t0 + n if t0 + n < s1 else None)
                    self.prenorm_T(Z["XA"], t0, 1, hT, bh[ntl:ntl + 1], TBG, l, which, xring, nring, rring, "2", halo=self._halo)
                    hb_all = [b for j in range(ntl) for b in bh[j]]
                    if GC < 3:
                        continue
                    for f in range(FC):
                        wu, bwu = wur.next()
                        self.load(wu[:, :, 0:128], wusrc[:, :, f * 128:(f + 1) * 128], [bwu], q="pool")
                        self.load(wu[:, :, 128:256], wusrc[:, :, DFF + f * 128:DFF + (f + 1) * 128], [bwu], q="pool")
                        pg, bpg = psg.next()
                        pu, bpu = psg.next()
                        ph, bph = self.PS[6]

                        def em(e):
                            r = None
                            for kc in range(KC):
                                r = e.matmul(pg[:, 0:n], lhsT=wu[:, kc, 0:128], rhs=hT[:, kc, 0:n], start=(kc == 0), stop=(kc == KC - 1))
                            return r
                        self.op("pe", em, reads=[bwu] + hb_all, writes=[bpg])

                        def em(e):
                            r = None
                            for kc in range(KC):
                                r = e.matmul(ph[:, 0:2], lhsT=wu[:, kc, 0:128], rhs=hT[:, kc, TBG:TBG + 2], start=(kc == 0), stop=(kc == KC - 1))
                            return r
                        self.op("pe", em, reads=[bwu, bh[ntl][0]], writes=[bph])

                        def em(e):
                            r = None
                            for kc in range(KC):
                                r = e.matmul(pu[:, 0:n], lhsT=wu[:, kc, 128:256], rhs=hT[:, kc, 0:n], start=(kc == 0), stop=(kc == KC - 1))
                            return r
                        self.op("pe", em, reads=[bwu] + hb_all, writes=[bpu])
                        gb_, bgb = gbr.next()
                        self.op("act", lambda e: e.activation(out=gb_[:, 1:n + 1], in_=pg[:, 0:n], func=AF.Copy), reads=[bpg], writes=[bgb])
                        if self._halo[0] is not None:
                            self.op("dve", lambda e: e.tensor_copy(out=gb_[:, 0:1], in_=ph[:, 0:1]), reads=[bph], writes=[bgb])
                        else:
                            self.op("dve", lambda e: e.memset(gb_[:, 0:1], 0.0), reads=[bph], writes=[bgb])
                        if self._halo[1] is not None:
                            self.op("dve", lambda e: e.tensor_copy(out=gb_[:, n + 1:n + 2], in_=ph[:, 1:2]), reads=[bph], writes=[bgb])
                        else:
                            self.op("dve", lambda e: e.memset(gb_[:, n + 1:n + 2], 0.0), reads=[bph], writes=[bgb])
                        t_, bt_ = tr_.next()
                        self.op("dve", lambda e: e.tensor_scalar(out=t_[:, 0:n], in0=gb_[:, 0:n], scalar1=cw[:, f:f + 1], scalar2=cw[:, 132 + f:133 + f], op0=ALU.mult, op1=ALU.add),
                                reads=[bgb, bcw], writes=[bt_])
                        self.op("dve", lambda e: e.scalar_tensor_tensor(out=t_[:, 0:n], in0=gb_[:, 1:n + 1], scalar=cw[:, 44 + f:45 + f], in1=t_[:, 0:n], op0=ALU.mult, op1=ALU.add),
                                reads=[bgb, bcw, bt_], writes=[bt_])
                        self.op("dve", lambda e: e.scalar_tensor_tensor(out=t_[:, 0:n], in0=gb_[:, 2:n + 2], scalar=cw[:, 88 + f:89 + f], in1=t_[:, 0:n], op0=ALU.mult, op1=ALU.add),
                                reads=[bgb, bcw, bt_], writes=[bt_])
                        self.op("act", lambda e: e.activation(out=t_[:, 0:n], in_=t_[:, 0:n], func=AF.Silu), reads=[bt_], writes=[bt_])
                        self.op("dve", lambda e: e.tensor_tensor(out=aT[:, f, 0:n], in0=pu[:, 0:n], in1=t_[:, 0:n], op=ALU.mult), reads=[bpu, bt_], writes=[ba[f]])
                    if GC < 4:
                        continue
                    for dc in range(D // 256):
                        wd, bwd = wdr.next()
                        for fq in range(4):
                            if os.environ.get("NOWD"):
                                continue
                            self.load(wd[:, fq * 11:(fq + 1) * 11, :], wdsrc[:, fq * 11:(fq + 1) * 11, dc * 256:(dc + 1) * 256], [bwd], q="pool")
                        for tt in range(ntl):
                            ps, bps = self.PS[6 + (dc * ntl + tt) % 2]

                            def em(e):
                                r = None
                                for f in range(FC):
                                    r = e.matmul(ps[:, 0:256], lhsT=aT[:, f, tt * 128:(tt + 1) * 128], rhs=wd[:, f, :], start=(f == 0), stop=(f == FC - 1))
                                return r
                            self.op("pe", em, reads=[bwd] + ba, writes=[bps])
                            j_, bj_ = jr.next()
                            e_, be_ = er.next()
                            self.op("dve", lambda e: e.tensor_copy(out=e_[:], in_=ps[:, 0:256]), reads=[bps], writes=[be_])
                            self.op("act", lambda e: e.activation(out=j_[:], in_=e_[:], func=AF.Square, accum_out=ssq[:, tt * 8 + dc:tt * 8 + dc + 1]), reads=[be_], writes=[bj_, bssq])
                            if not os.environ.get("NOST"):
                                self.store(Z["RAW"][t0 + tt * 128:t0 + (tt + 1) * 128, dc * 256:(dc + 1) * 256], e_[:], [be_])
                    S.barrier()
                    if GC < 5:
                        continue
                    for tt in range(ntl):
                        tsl = slice(t0 + tt * 128, t0 + (tt + 1) * 128)
                        r_, br_ = rring.next()
                        self.op("dve", lambda e: e.tensor_reduce(out=r_[:, 0:1], in_=ssq[:, tt * 8:tt * 8 + 8], axis=AX.X, op=ALU.add), reads=[bssq], writes=[br_])
                        self.op("act", lambda e: e.activation(out=r_[:, 1:2], in_=r_[:, 0:1], func=AF.Sqrt, scale=1.0 / D, bias=EPS), reads=[br_], writes=[br_])
                        self.op("dve", lambda e: e.reciprocal(out=r_[:, 2:3], in_=r_[:, 1:2]), reads=[br_], writes=[br_])
                        raw, braw = xring.next()
                        self.load(raw[:], Z["RAW"][tsl, :], [braw])
                        x, bx = xring.next()
                        self.load(x[:], Z["XA"][tsl, :], [bx])
                        self.op("dve", lambda e: e.scalar_tensor_tensor(out=raw[:], in0=raw[:], scalar=r_[:, 2:3], in1=G[:, which, :], op0=ALU.mult, op1=ALU.mult),
                                reads=[braw, br_, bG], writes=[braw])
                        self.op("pool", lambda e: e.tensor_tensor(out=raw[:], in0=raw[:], in1=x[:], op=ALU.add), reads=[braw, bx], writes=[braw])
                        if last:
                            self.store(self.out[tsl.start - TC:tsl.stop - TC, :], raw[:], [braw])
                        else:
                            self.store(Z["XB"][tsl, :], raw[:], [braw])
            S.barrier()


def rope_table():
    t = np.arange(TL)
    row = (t // 64).astype(np.float32)
    col = (t % 64).astype(np.float32)
    inv = np.power(np.float32(10000.0), -np.arange(0, 64, 2, dtype=np.float32) / np.float32(64)).astype(np.float32)
    ang = np.concatenate([row[:, None] * inv, col[:, None] * inv], axis=-1).astype(np.float32)
    return np.ascontiguousarray(np.concatenate([np.cos(ang), np.sin(ang)], axis=-1).astype(np.float32))


PARAMS = ["w_ada", "b_ada", "pre_mix_g", "post_mix_g", "w_in", "ml_gate_b", "ml_norm_g", "sg_ln_g", "sg_ln_b", "sg_w",
          "sg_b", "cv_w", "cv_b", "cv_ln_g", "cv_ln_b", "at_qn_g", "at_kn_g", "w_out", "pre_ffn_g", "post_ffn_g", "w_up",
          "ffn_cv_w", "ffn_cv_b", "w_down"]


def make_in_map(inputs, b, rope):
    m = {"xin": np.ascontiguousarray(np.concatenate([inputs["ctx"][b], inputs["x"][b]], axis=0), dtype=np.float32),
         "cvec": np.ascontiguousarray(np.stack([inputs["c"][b], inputs["c_ctx"]], axis=0), dtype=np.float32),
         "rope": rope}
    for p in PARAMS:
        m[p] = np.ascontiguousarray(inputs[p], dtype=np.float32)
    return m


def kernel(**inputs):
    inputs = {k: np.asarray(v) for k, v in inputs.items()}
    kb = K()
    rope = rope_table()
    maps = [make_in_map(inputs, c % 4, rope) for c in range(NCORES)]
    res = run_bass_kernel_spmd(kb.nc, maps, core_ids=list(range(NCORES)))
    out = np.stack([np.asarray(res.results[b]["out"], dtype=np.float32) for b in range(4)], axis=0)
    return out
```

```python
import contextlib
import os
CUT = int(os.environ.get('CUTA', '99'))
import numpy as np
import concourse.bass as bass
import concourse.mybir as mybir
from concourse.bass_utils import run_bass_kernel_spmd

F32 = mybir.dt.float32
BF16 = mybir.dt.bfloat16
AF = mybir.ActivationFunctionType
ALU = mybir.AluOpType
AX = mybir.AxisListType

D = 2048
DIN = 5136
DFF = 5632
TC = 256
TL = 4096
T = TC + TL
NT = T // 128
EPS = 1e-6
KC = D // 128
FC = DFF // 128
NCORES = 8


class Buf:
    __slots__ = ("name", "w", "r")

    def __init__(self, name=""):
        self.name = name
        self.w = None
        self.r = {}


class Sched:
    KD = 8

    def __init__(self, nc):
        self.nc = nc
        self.E = {"pe": nc.tensor, "dve": nc.vector, "act": nc.scalar, "pool": nc.gpsimd, "sp": nc.sync}
        self.sem = {}
        self.cnt = {}
        for e in ["pe", "dve", "act", "pool"]:
            self.sem[e] = nc.alloc_semaphore("s_" + e)
            self.cnt[e] = 0
        self.dq = {}
        for q in ["sp", "act", "pool"]:
            self.dq[q] = {"n": 0, "sems": [nc.alloc_semaphore(f"d_{q}{i}") for i in range(self.KD)],
                          "tot": [0] * self.KD}
            for i in range(self.KD):
                self.sem[(q, i)] = self.dq[q]["sems"][i]
        self.known = {e: {} for e in self.E}

    def _wait(self, eng, key, val):
        if val <= 0:
            return
        k = self.known[eng]
        if k.get(key, 0) >= val:
            return
        self.E[eng].wait_ge(self.sem[key], val)
        k[key] = val

    def _deps(self, eng, reads, writes):
        need = {}
        for b in reads:
            if b.w is not None:
                need[b.w[0]] = max(need.get(b.w[0], 0), b.w[1])
        for b in writes:
            if b.w is not None:
                need[b.w[0]] = max(need.get(b.w[0], 0), b.w[1])
            for k, v in b.r.items():
                need[k] = max(need.get(k, 0), v)
        for k, v in need.items():
            if k == "pe" and eng == "pe":
                continue
            self._wait(eng, k, v)

    def _mark(self, tok, reads, writes):
        for b in reads:
            b.r[tok[0]] = max(b.r.get(tok[0], 0), tok[1])
        for b in writes:
            b.w = tok
            b.r = {}

    def op(self, eng, emit, reads=(), writes=()):
        self._deps(eng, reads, writes)
        inst = emit(self.E[eng])
        self.cnt[eng] += 1
        inst.then_inc(self.sem[eng], 1)
        self._mark((eng, self.cnt[eng]), reads, writes)

    def dma(self, q, out, in_, reads=(), writes=(), **kw):
        self._deps(q, reads, writes)
        d = self.dq[q]
        i = d["n"] % self.KD
        d["n"] += 1
        self._wait(q, (q, i), d["tot"][i])
        self.E[q].dma_start(out=out, in_=in_, **kw).then_inc(d["sems"][i], 16)
        d["tot"][i] += 16
        self._mark(((q, i), d["tot"][i]), reads, writes)

    def barrier(self):
        tot = {}
        for e in ["pe", "dve", "act", "pool"]:
            tot[e] = self.cnt[e]
        for q in self.dq:
            for i in range(self.KD):
                tot[(q, i)] = self.dq[q]["tot"][i]
        for e in self.E:
            for k, v in tot.items():
                if k == e:
                    continue
                self._wait(e, k, v)


class Ring:
    def __init__(self, tiles):
        self.tiles = tiles
        self.i = 0

    def next(self):
        t = self.tiles[self.i % len(self.tiles)]
        self.i += 1
        return t


class K:
    def __init__(self, debug=None, nlayers=2, stop_after=None):
        self.debug = debug or ()
        self.nlayers = nlayers
        self.stop_after = stop_after
        nc = self.nc = bass.Bass("TRN2", target_bir_lowering=False)
        self.S = Sched(nc)
        self.uid = 0
        self.outs = []
        self.build()

    def name(self, p):
        self.uid += 1
        return f"{p}_{self.uid}"

    def din(self, name, shape, dt=F32):
        return self.nc.dram_tensor(name, list(shape), dt, kind="ExternalInput").ap()

    def scratch(self, name, shape, dt):
        if name in self.debug:
            self.outs.append(name)
            return self.nc.dram_tensor(name, list(shape), dt, kind="ExternalOutput").ap()
        return self.nc.dram_tensor(name, list(shape), dt, kind="Internal").ap()

    def sb(self, stack, p, shape, dt):
        t = stack.enter_context(self.nc.sbuf_tensor(self.name(p), list(shape), dt))
        return t, Buf(p)

    def ring(self, stack, p, shape, dt, n):
        return Ring([self.sb(stack, p, shape, dt) for _ in range(n)])

    def op(self, eng, emit, reads=(), writes=()):
        self.S.op(eng, emit, reads, writes)

    def load(self, out, in_, writes, q="sp", reads=()):
        self.S.dma(q, out, in_, reads=reads, writes=writes)

    def store(self, out, in_, reads, q="sp"):
        self.S.dma(q, out, in_, reads=reads, writes=())

    def load_fm(self, stack, dst_ap, dst_buf, src_rows_ap, R):
        st, bst = self.stage_fm
        self.load(st[0:R, :], src_rows_ap, [bst])
        ps, bps = self.PS[7]
        self.op("pe", lambda e: e.transpose(out=ps[:, 0:R], in_=st[0:R, :], identity=self.ident[0:R, 0:R]),
                reads=[bst, self.bconst], writes=[bps])
        self.op("dve", lambda e: e.tensor_copy(out=dst_ap, in_=ps[:, 0:R]), reads=[bps], writes=[dst_buf])

    def build(self):
        nc = self.nc
        S = self.S
        I = self.I = {}
        I["xin"] = self.din("xin", [T, D])
        I["cvec"] = self.din("cvec", [2, D])
        I["rope"] = self.din("rope", [TL, 128])
        I["w_ada"] = self.din("w_ada", [2, D, 6 * D])
        I["b_ada"] = self.din("b_ada", [2, 6 * D])
        for n, sh in [("pre_mix_g", [2, D]), ("post_mix_g", [2, D]), ("w_in", [2, D, DIN]), ("ml_gate_b", [2, 16]),
                      ("ml_norm_g", [2, 512]), ("sg_ln_g", [2, 512]), ("sg_ln_b", [2, 512]), ("sg_w", [2, 4, 128, 128]),
                      ("sg_b", [2, 4, 128]), ("cv_w", [2, 31, 512]), ("cv_b", [2, 512]), ("cv_ln_g", [2, 512]),
                      ("cv_ln_b", [2, 512]), ("at_qn_g", [2, 128]), ("at_kn_g", [2, 128]), ("w_out", [2, D, D]),
                      ("pre_ffn_g", [2, D]), ("post_ffn_g", [2, D]), ("w_up", [2, D, 2 * DFF]),
                      ("ffn_cv_w", [2, 3, DFF]), ("ffn_cv_b", [2, DFF]), ("w_down", [2, DFF, D])]:
            I[n] = self.din(n, sh)
        self.out = nc.dram_tensor("out", [TL, D], F32, kind="ExternalOutput").ap()

        Z = self.Z = {}
        Z["XA"] = self.scratch("XA", [T, D], F32)
        Z["XB"] = self.scratch("XB", [T, D], F32)
        Z["MQT"] = self.scratch("MQT", [4, 128, T], BF16)
        Z["MKT"] = self.scratch("MKT", [4, 128, T], BF16)
        Z["MK"] = self.scratch("MK", [T, 512], BF16)
        Z["MV"] = self.scratch("MV", [T, 512], BF16)
        Z["MO"] = self.scratch("MO", [T, 512], BF16)
        Z["MG"] = self.scratch("MG", [T, 16], F32)
        Z["SU"] = self.scratch("SU", [4, 128, T], BF16)
        Z["SV"] = self.scratch("SV", [T, 512], BF16)
        Z["CY"] = self.scratch("CY", [4, 128, T], F32)
        Z["AQT"] = self.scratch("AQT", [4, 128, T], BF16)
        Z["AKT"] = self.scratch("AKT", [2, 128, T], BF16)
        Z["AV"] = self.scratch("AV", [T, 256], BF16)
        Z["YT"] = self.scratch("YT", [16, 128, T], BF16)
        Z["HF"] = self.scratch("HF", [T, 512], F32)
        Z["GV"] = self.scratch("GV", [2, 2, 2, D], F32)
        Z["RAW"] = self.scratch("RAW", [T, D], F32)
        Z["WUS"] = self.scratch("WUS", [FC, 128, KC * 256], BF16)
        Z["WDS"] = self.scratch("WDS", [D // 256, 128, FC * 256], BF16)

        with contextlib.ExitStack() as gs:
            self.PS = []
            for i in range(8):
                if i < 2:
                    t = gs.enter_context(nc.psum_tensor(f"ps{i}", [128, 1024], BF16))
                else:
                    t = gs.enter_context(nc.psum_tensor(f"ps{i}", [128, 512], F32))
                self.PS.append((t, Buf(f"ps{i}")))
            self.bconst = Buf("const")
            identf = gs.enter_context(nc.sbuf_tensor("identf", [128, 128], F32))
            identb = gs.enter_context(nc.sbuf_tensor("identb", [128, 128], BF16))
            onesf = gs.enter_context(nc.sbuf_tensor("onesf", [128, 128], F32))
            onesb = gs.enter_context(nc.sbuf_tensor("onesb", [128, 128], BF16))
            mkF = gs.enter_context(nc.sbuf_tensor("mkF", [128, 128], F32))
            mkB = gs.enter_context(nc.sbuf_tensor("mkB", [128, 128], F32))
            self.ident, self.identb, self.onesf, self.onesb, self.mkF, self.mkB = identf, identb, onesf, onesb, mkF, mkB
            bc = self.bconst
            self.op("pool", lambda e: e.memset(identf[:], 0.0), writes=[bc])
            self.op("pool", lambda e: e.affine_select(out=identf[:], in_=identf[:], pattern=[[-1, 128]],
                                                      compare_op=ALU.not_equal, fill=1.0, base=0, channel_multiplier=1),
                    reads=[bc], writes=[bc])
            self.op("pool", lambda e: e.memset(onesf[:], 1.0), writes=[bc])
            self.op("pool", lambda e: e.affine_select(out=mkF[:], in_=onesf[:], pattern=[[1, 128]],
                                                      compare_op=ALU.is_ge, fill=0.0, base=0, channel_multiplier=-1),
                    reads=[bc], writes=[bc])
            self.op("pool", lambda e: e.affine_select(out=mkB[:], in_=onesf[:], pattern=[[-1, 128]],
                                                      compare_op=ALU.is_ge, fill=0.0, base=0, channel_multiplier=1),
                    reads=[bc], writes=[bc])
            self.op("dve", lambda e: e.tensor_copy(out=identb[:], in_=identf[:]), reads=[bc], writes=[bc])
            self.op("dve", lambda e: e.tensor_copy(out=onesb[:], in_=onesf[:]), reads=[bc], writes=[bc])
            st = gs.enter_context(nc.sbuf_tensor("stage_fm", [128, 128], F32))
            self.stage_fm = (st, Buf("stage_fm"))
            self.mod = [[{}, {}] for _ in range(2)]
            self.bmod = Buf("mod")
            for l in range(2):
                for w in range(2):
                    for nm in ["gsc1", "sh1", "gsc2", "sh2"]:
                        self.mod[l][w][nm] = gs.enter_context(nc.sbuf_tensor(f"mod_{l}_{w}_{nm}", [128, KC], F32))

            for l in range(self.nlayers):
                self.phase0(l)
            if self.stop_after == "0":
                S.barrier()
                return
            for l in range(self.nlayers):
                last = (l == 1)
                xsrc = I["xin"] if l == 0 else Z["XB"]
                self.phaseA(l, xsrc, last)
                if self.stop_after == "A":
                    break
                stop = False
                for nm, fn in [("B", self.phaseB), ("C", self.phaseC), ("D", self.phaseD), ("E", self.phaseE)]:
                    only = os.environ.get("ONLY")
                    if only is None or nm in only:
                        fn(l, last)
                    if self.stop_after == nm:
                        stop = True
                        break
                if stop:
                    break
                self.phaseF(l, xsrc, last)
                if self.stop_after == "F":
                    break
                self.phaseG(l, last)
            S.barrier()

    def phase0(self, l):
        nc, S, I, Z = self.nc, self.S, self.I, self.Z
        with contextlib.ExitStack() as st:
            sstage, bss = self.sb(st, "sstage", [32, 128], F32)
            self.load(sstage[0:16, :], I["cvec"][0].rearrange("(k p) -> k p", p=128), [bss])
            self.load(sstage[16:32, :], I["cvec"][1].rearrange("(k p) -> k p", p=128), [bss])
            self.op("act", lambda e: e.activation(out=sstage[:], in_=sstage[:], func=AF.Silu), reads=[bss], writes=[bss])
            ps, bps = self.PS[7]
            self.op("pe", lambda e: e.transpose(out=ps[:, 0:32], in_=sstage[:], identity=self.ident[0:32, 0:32]),
                    reads=[bss, self.bconst], writes=[bps])
            sT, bsT = self.sb(st, "sT", [128, KC, 2], BF16)
            self.op("dve", lambda e: e.tensor_copy(out=sT[:].rearrange("p k w -> p w k"),
                                                   in_=ps[:, 0:32].rearrange("p (w k) -> p w k", w=2)),
                    reads=[bps], writes=[bsT])
            srep, bsr = self.sb(st, "srep", [128, 2, KC, 128], BF16)
            for w in range(2):
                self.op("dve", lambda e, w=w: e.tensor_copy(out=srep[:, w, :, :],
                                                            in_=sT[:, :, w:w + 1].to_broadcast([128, KC, 128])),
                        reads=[bsT], writes=[bsr])
            bada, bbada = self.sb(st, "bada", [1, 6 * D], F32)
            self.load(bada[:], I["b_ada"][l:l + 1, :], [bbada])
            badab, bbadab = self.sb(st, "badab", [1, 6 * D], BF16)
            self.op("dve", lambda e: e.tensor_copy(out=badab[:], in_=bada[:]), reads=[bbada], writes=[bbadab])
            gfm, bgfm = self.sb(st, "gfm", [128, 2, KC], F32)
            self.load_fm(st, gfm[:, 0, :], bgfm, I["pre_mix_g"][l].rearrange("(k p) -> k p", p=128), KC)
            self.load_fm(st, gfm[:, 1, :], bgfm, I["pre_ffn_g"][l].rearrange("(k p) -> k p", p=128), KC)
            pgb, bpgb = self.sb(st, "pgb", [128, 2, D], F32)
            self.load(pgb[:, 0, :], I["post_mix_g"][l].partition_broadcast(128), [bpgb])
            self.load(pgb[:, 1, :], I["post_ffn_g"][l].partition_broadcast(128), [bpgb])
            wring = self.ring(st, "wada", [128, KC, 512], BF16, 3)
            wsrc = I["w_ada"][l].rearrange("(k p) c -> p k c", p=128)
            pm, bpm = self.PS[6]
            gstage = self.ring(st, "gstage", [128, 512], F32, 2)
            for blk in range(24):
                seg = blk // 4
                wt, bwt = wring.next()
                self.load(wt[:], wsrc[:, :, blk * 512:(blk + 1) * 512], [bwt], q="pool")
                if seg in (2, 5):
                    gi = 0 if seg == 2 else 1
                    for w in range(2):
                        pg, bpg = self.PS[2 + (blk * 2 + w) % 4]

                        def em(e, w=w, pg=pg, wt=wt, blk=blk):
                            for kc in range(KC):
                                e.matmul(pg[:], lhsT=srep[:, w, kc, :], rhs=wt[:, kc, :], start=(kc == 0), stop=False)
                            return e.matmul(pg[:], lhsT=self.onesb[0:1, :], rhs=badab[0:1, blk * 512:(blk + 1) * 512],
                                            start=False, stop=True)
                        self.op("pe", em, reads=[bsr, bwt, bbadab, self.bconst], writes=[bpg])
                        gt, bgt = gstage.next()
                        c0 = (blk % 4) * 512
                        self.op("dve", lambda e, pg=pg, gt=gt, gi=gi, c0=c0: e.tensor_tensor(
                            out=gt[:], in0=pg[:], in1=pgb[:, gi, c0:c0 + 512], op=ALU.mult), reads=[bpg, bpgb], writes=[bgt])
                        self.store(Z["GV"][l, w, gi:gi + 1, c0:c0 + 512], gt[0:1, :], [bgt])
                else:
                    def em(e, wt=wt, blk=blk):
                        r = None
                        for j in range(4):
                            cc = blk * 4 + j
                            for kc in range(KC):
                                e.matmul(pm[:, cc * 2:cc * 2 + 2], lhsT=wt[:, kc, j * 128:(j + 1) * 128], rhs=sT[:, kc, :],
                                         start=(kc == 0), stop=False)
                            r = e.matmul(pm[:, cc * 2:cc * 2 + 2], lhsT=badab[0:1, cc * 128:(cc + 1) * 128],
                                         rhs=self.onesb[0:1, 0:2], start=False, stop=True)
                        return r
                    self.op("pe", em, reads=[bsT, bwt, bbadab, self.bconst], writes=[bpm])
            pmv = pm[:, 0:192].rearrange("p (c w) -> p c w", w=2)
            for w in range(2):
                m = self.mod[l][w]
                for nm, sc_seg, sh_seg, gi in [("1", 1, 0, 0), ("2", 4, 3, 1)]:
                    self.op("dve", lambda e, m=m, nm=nm, sc_seg=sc_seg, gi=gi, w=w: e.scalar_tensor_tensor(
                        out=m["gsc" + nm][:], in0=pmv[:, sc_seg * 16:(sc_seg + 1) * 16, w], scalar=1.0, in1=gfm[:, gi, :],
                        op0=ALU.add, op1=ALU.mult), reads=[bpm, bgfm], writes=[self.bmod])
                    self.op("dve", lambda e, m=m, nm=nm, sh_seg=sh_seg, w=w: e.tensor_copy(
                        out=m["sh" + nm][:], in_=pmv[:, sh_seg * 16:(sh_seg + 1) * 16, w]), reads=[bpm], writes=[self.bmod])
            S.barrier()

    def prenorm_T(self, xsrc, t0, ntiles, hT, bhT, col0, l, which, xring, nring, rring, key, halo=None):
        m = self.mod[l][which]
        gsc, sh = m["gsc" + key], m["sh" + key]
        for j in range(ntiles):
            xt, bxt = xring.next()
            if halo is None:
                self.load(xt[:], xsrc[t0 + j * 128:t0 + (j + 1) * 128, :], [bxt])
            else:
                self.op("pool", lambda e, xt=xt: e.memset(xt[:], 0.0), writes=[bxt])
                for hi_, tk in enumerate(halo):
                    if tk is not None:
                        self.load(xt[hi_:hi_ + 1, :], xsrc[tk:tk + 1, :], [bxt])
            xn, bxn = nring.next()
            rs, brs = rring.next()
            self.op("act", lambda e, xn=xn, xt=xt, rs=rs: e.activation(out=xn[:], in_=xt[:], func=AF.Square, accum_out=rs[:, 0:1]),
                    reads=[bxt], writes=[bxn, brs])
            self.op("act", lambda e, rs=rs: e.activation(out=rs[:, 1:2], in_=rs[:, 0:1], func=AF.Sqrt, scale=1.0 / D, bias=EPS),
                    reads=[brs], writes=[brs])
            self.op("dve", lambda e, rs=rs: e.reciprocal(out=rs[:, 2:3], in_=rs[:, 1:2]), reads=[brs], writes=[brs])
            self.op("dve", lambda e, xn=xn, xt=xt, rs=rs: e.tensor_scalar(out=xn[:], in0=xt[:], scalar1=rs[:, 2:3], scalar2=None,
                                                                     op0=ALU.mult), reads=[bxt, brs], writes=[bxn])
            for half in range(2):
                pt, bpt = self.PS[half]
                ptb = pt

                def em(e, half=half, ptb=ptb, xn=xn):
                    r = None
                    for k in range(8):
                        kc = half * 8 + k
                        r = e.transpose(out=ptb[:, k * 128:(k + 1) * 128], in_=xn[:, kc * 128:(kc + 1) * 128], identity=self.identb[:])
                    return r
                self.op("pe", em, reads=[bxn, self.bconst], writes=[bpt])
                for k in range(8):
                    kc = half * 8 + k
                    eng = "act"
                    dst = hT[:, kc, col0 + j * 128:col0 + (j + 1) * 128]
                    if eng == "act":
                        self.op("act", lambda e, dst=dst, ptb=ptb, k=k, kc=kc: e.activation(
                            out=dst, in_=ptb[:, k * 128:(k + 1) * 128], func=AF.Identity, scale=gsc[:, kc:kc + 1], bias=sh[:, kc:kc + 1]),
                            reads=[bpt, self.bmod], writes=[bhT[j][0]])
                    else:
                        self.op("dve", lambda e, dst=dst, ptb=ptb, k=k, kc=kc: e.tensor_scalar(
                            out=dst, in0=ptb[:, k * 128:(k + 1) * 128], scalar1=gsc[:, kc:kc + 1], scalar2=sh[:, kc:kc + 1],
                            op0=ALU.mult, op1=ALU.add), reads=[bpt, self.bmod], writes=[bhT[j][1]])

    def phaseA(self, l, xsrc, last):
        nc, S, I, Z = self.nc, self.S, self.I, self.Z
        TBA = 1024
        with contextlib.ExitStack() as st:
            hT, _ = self.sb(st, "hT", [128, KC, TBA], BF16)
            bh = [(Buf(), Buf()) for _ in range(TBA // 128)]
            xring = self.ring(st, "xa", [128, D], F32, 2)
            nring = self.ring(st, "xn", [128, D], BF16, 2)
            rring = self.ring(st, "rs", [128, 4], F32, 3)
            wring = self.ring(st, "win", [128, KC, 528], BF16, 3)
            wsrc = I["w_in"][l].rearrange("(k p) c -> p k c", p=128)
            cst, bcst = self.sb(st, "cstA", [128, 16 + 512 + 512 + 128 + 128], F32)
            gb = cst[:, 0:16]
            sgg = cst[:, 16:528]
            sgbb = cst[:, 528:1040]
            qg = cst[:, 1040:1168]
            kg = cst[:, 1168:1296]
            self.load(gb, I["ml_gate_b"][l].partition_broadcast(128), [bcst])
            self.load(sgg, I["sg_ln_g"][l].partition_broadcast(128), [bcst])
            self.load(sgbb, I["sg_ln_b"][l].partition_broadcast(128), [bcst])
            self.load(qg, I["at_qn_g"][l].partition_broadcast(128), [bcst])
            self.load(kg, I["at_kn_g"][l].partition_broadcast(128), [bcst])
            ef = self.ring(st, "ef", [128, 512], F32, 4)
            eb = self.ring(st, "eb", [128, 512], BF16, 4)
            sm = self.ring(st, "sm", [128, 16], F32, 4)
            csr = self.ring(st, "cs", [128, 128], F32, 2)
            psring = Ring(self.PS[2:8])
            cnt = [0]

            def copy_op(dst, src, reads, writes):
                cnt[0] += 1
                if cnt[0] % 2:
                    self.op("act", lambda e: e.activation(out=dst, in_=src, func=AF.Copy), reads=reads, writes=writes)
                else:
                    self.op("dve", lambda e: e.tensor_copy(out=dst, in_=src), reads=reads, writes=writes)

            blocks = [(0, 2, 1)] + [(TC + i * TBA, TBA // 128, 0) for i in range(TL // TBA)]
            for (t0, ntl, which) in blocks:
                n = ntl * 128
                if CUT < 0:
                    continue
                self.prenorm_T(xsrc, t0, ntl, hT, bh, 0, l, which, xring, nring, rring, "1")
                subs = [(s0, min(512, n - s0)) for s0 in range(0, n, 512)]

                def getpanel(c0, w):
                    wt, bwt = wring.next()
                    self.load(wt[:, :, 0:w], wsrc[:, :, c0:c0 + w], [bwt], q="pool")
                    return wt, bwt

                def fm_mm(wt, bwt, coff, t_lo, nn):
                    ps, bps = psring.next()

                    def em(e):
                        r = None
                        for kc in range(KC):
                            r = e.matmul(ps[:, 0:nn], lhsT=wt[:, kc, coff:coff + 128], rhs=hT[:, kc, t_lo:t_lo + nn],
                                         start=(kc == 0), stop=(kc == KC - 1))
                        return r
                    rb = [b for j in range(t_lo // 128, (t_lo + nn) // 128) for b in bh[j]]
                    self.op("pe", em, reads=[bwt] + rb, writes=[bps])
                    return ps, bps

                def tm_mm(wt, bwt, coff, ncols, j):
                    ps, bps = psring.next()

                    def em(e):
                        r = None
                        for kc in range(KC):
                            r = e.matmul(ps[:, 0:ncols], lhsT=hT[:, kc, j * 128:(j + 1) * 128], rhs=wt[:, kc, coff:coff + ncols],
                                         start=(kc == 0), stop=(kc == KC - 1))
                        return r
                    self.op("pe", em, reads=[bwt, bh[j][0], bh[j][1]], writes=[bps])
                    return ps, bps

                def fm_simple(c0, dst, func=None):
                    wt, bwt = getpanel(c0, 512)
                    for ch in range(4):
                        for (s0, nn) in subs:
                            ps, bps = fm_mm(wt, bwt, ch * 128, s0, nn)
                            o, bo = eb.next()
                            if func is None:
                                copy_op(o[:, 0:nn], ps[:, 0:nn], [bps], [bo])
                            else:
                                self.op("act", lambda e, o=o, ps=ps, nn=nn: e.activation(out=o[:, 0:nn], in_=ps[:, 0:nn], func=func),
                                        reads=[bps], writes=[bo])
                            self.store(dst[ch, :, t0 + s0:t0 + s0 + nn], o[:, 0:nn], [bo])
                    return wt, bwt

                def tm_simple(wt, bwt, coff, ncols, dst, func=None):
                    for j in range(ntl):
                        ps, bps = tm_mm(wt, bwt, coff, ncols, j)
                        o, bo = eb.next()
                        if func is None:
                            copy_op(o[:, 0:ncols], ps[:, 0:ncols], [bps], [bo])
                        else:
                            self.op("act", lambda e, o=o, ps=ps: e.activation(out=o[:, 0:ncols], in_=ps[:, 0:ncols], func=func),
                                    reads=[bps], writes=[bo])
                        self.store(dst[t0 + j * 128:t0 + (j + 1) * 128, :], o[:, 0:ncols], [bo])

                if CUT < 1:
                    continue
                fm_simple(0, Z["MQT"])
                if CUT < 2:
                    continue
                wt, bwt = fm_simple(512, Z["MKT"])
                tm_simple(wt, bwt, 0, 512, Z["MK"])
                if CUT < 3:
                    continue
                wt, bwt = getpanel(1024, 512)
                tm_simple(wt, bwt, 0, 512, Z["MV"])
                wt, bwt = getpanel(1536, 528)
                tm_simple(wt, bwt, 0, 512, Z["MO"], func=AF.Sigmoid)
                if CUT < 4:
                    continue
                for j in range(ntl):
                    ps, bps = tm_mm(wt, bwt, 512, 16, j)
                    o, bo = sm.next()
                    self.op("dve", lambda e, o=o, ps=ps: e.tensor_tensor(out=o[:], in0=ps[:, 0:16], in1=gb, op=ALU.add),
                            reads=[bps, bcst], writes=[bo])
                    self.store(Z["MG"][t0 + j * 128:t0 + (j + 1) * 128, :], o[:], [bo])
                if CUT < 5:
                    continue
                fm_simple(2064, Z["SU"], func=AF.Gelu_apprx_tanh)
                if CUT < 6:
                    continue
                wt, bwt = getpanel(2576, 512)
                for j in range(ntl):
                    ps, bps = tm_mm(wt, bwt, 0, 512, j)
                    g, bg = ef.next()
                    self.op("act", lambda e, g=g, ps=ps: e.activation(out=g[:], in_=ps[:], func=AF.Gelu_apprx_tanh), reads=[bps], writes=[bg])
                    s6, bs6 = sm.next()
                    self.op("dve", lambda e, s6=s6, g=g: e.bn_stats(out=s6[:, 0:6], in_=g[:]), reads=[bg], writes=[bs6])
                    self.op("dve", lambda e, s6=s6: e.bn_aggr(out=s6[:, 8:10], in_=s6[:, 0:6]), reads=[bs6], writes=[bs6])
                    self.op("act", lambda e, s6=s6: e.activation(out=s6[:, 10:11], in_=s6[:, 9:10], func=AF.Sqrt, scale=1.0, bias=EPS),
                            reads=[bs6], writes=[bs6])
                    self.op("dve", lambda e, s6=s6: e.reciprocal(out=s6[:, 11:12], in_=s6[:, 10:11]), reads=[bs6], writes=[bs6])
                    self.op("dve", lambda e, s6=s6, g=g: e.tensor_scalar(out=g[:], in0=g[:], scalar1=s6[:, 8:9], scalar2=s6[:, 11:12],
                                                                    op0=ALU.subtract, op1=ALU.mult), reads=[bg, bs6], writes=[bg])
                    self.op("pool", lambda e, g=g: e.tensor_tensor(out=g[:], in0=g[:], in1=sgg, op=ALU.mult), reads=[bg, bcst], writes=[bg])
                    o, bo = eb.next()
                    self.op("pool", lambda e, g=g, o=o: e.tensor_tensor(out=o[:], in0=g[:], in1=sgbb, op=ALU.add), reads=[bg, bcst], writes=[bo])
                    self.store(Z["SV"][t0 + j * 128:t0 + (j + 1) * 128, :], o[:], [bo])
                if CUT < 7:
                    continue
                wa, bwa = getpanel(3088, 512)
                wg, bwg = getpanel(3600, 512)
                for ch in range(4):
                    for (s0, nn) in subs:
                        pa, bpa = fm_mm(wa, bwa, ch * 128, s0, nn)
                        pg, bpg = fm_mm(wg, bwg, ch * 128, s0, nn)
                        g, bg = ef.next()
                        self.op("act", lambda e, g=g, pg=pg, nn=nn: e.activation(out=g[:, 0:nn], in_=pg[:, 0:nn], func=AF.Sigmoid),
                                reads=[bpg], writes=[bg])
                        o, bo = ef.next()
                        self.op("dve", lambda e, o=o, pa=pa, g=g, nn=nn: e.tensor_tensor(out=o[:, 0:nn], in0=pa[:, 0:nn], in1=g[:, 0:nn], op=ALU.mult),
                                reads=[bpa, bg], writes=[bo])
                        self.store(Z["CY"][ch, :, t0 + s0:t0 + s0 + nn], o[:, 0:nn], [bo])
                def qk_path(wt, bwt, coff, nh, gain, dst):
                    for j in range(ntl):
                        ps, bps = tm_mm(wt, bwt, coff, nh * 128, j)
                        w_ = nh * 128
                        g, bg = ef.next()
                        self.op("act", lambda e, g=g, ps=ps: e.activation(out=g[:, 0:w_], in_=ps[:, 0:w_], func=AF.Square), reads=[bps], writes=[bg])
                        s6, bs6 = sm.next()
                        self.op("dve", lambda e, s6=s6, g=g: e.tensor_reduce(out=s6[:, 0:nh], in_=g[:, 0:w_].rearrange("p (h d) -> p h d", h=nh),
                                                                        axis=AX.X, op=ALU.add), reads=[bg], writes=[bs6])
                        self.op("act", lambda e, s6=s6: e.activation(out=s6[:, 4:4 + nh], in_=s6[:, 0:nh], func=AF.Sqrt, scale=1.0 / 128, bias=EPS),
                                reads=[bs6], writes=[bs6])
                        self.op("dve", lambda e, s6=s6: e.reciprocal(out=s6[:, 8:8 + nh], in_=s6[:, 4:4 + nh]), reads=[bs6], writes=[bs6])
                        q3 = g[:, 0:w_].rearrange("p (h d) -> p h d", h=nh)
                        self.op("dve", lambda e, q3=q3, ps=ps, s6=s6: e.tensor_tensor(
                            out=q3, in0=ps[:, 0:w_].rearrange("p (h d) -> p h d", h=nh),
                            in1=s6[:, 8:8 + nh].unsqueeze(2).to_broadcast([128, nh, 128]), op=ALU.mult), reads=[bps, bs6], writes=[bg])
                        qr, bqr = eb.next()
                        qr3 = qr[:, 0:w_].rearrange("p (h d) -> p h d", h=nh)
                        gn3 = gain.unsqueeze(1).to_broadcast([128, nh, 128])
                        if which == 0:
                            self.op("pool", lambda e, q3=q3: e.tensor_tensor(out=q3, in0=q3, in1=gn3, op=ALU.mult), reads=[bg, bcst], writes=[bg])
                            cs, bcs = csr.next()
                            tl0 = t0 - TC + j * 128
                            self.load(cs[:], I["rope"][tl0:tl0 + 128, :], [bcs])
                            C3 = cs[:, 0:64].unsqueeze(1).to_broadcast([128, nh, 64])
                            S3 = cs[:, 64:128].unsqueeze(1).to_broadcast([128, nh, 64])
                            t, bt = ef.next()
                            t3 = t[:, 0:w_].rearrange("p (h d) -> p h d", h=nh)
                            x1, x2 = q3[:, :, 0:64], q3[:, :, 64:128]
                            self.op("dve", lambda e: e.tensor_tensor(out=t3[:, :, 0:64], in0=x1, in1=C3, op=ALU.mult), reads=[bg, bcs], writes=[bt])
                            self.op("pool", lambda e: e.tensor_tensor(out=t3[:, :, 64:128], in0=x2, in1=S3, op=ALU.mult), reads=[bg, bcs], writes=[bt])
                            self.op("dve", lambda e: e.tensor_tensor(out=qr3[:, :, 0:64], in0=t3[:, :, 0:64], in1=t3[:, :, 64:128], op=ALU.subtract),
                                    reads=[bt], writes=[bqr])
                            t_, bt_ = ef.next()
                            u3 = t_[:, 0:w_].rearrange("p (h d) -> p h d", h=nh)
                            self.op("dve", lambda e: e.tensor_tensor(out=u3[:, :, 0:64], in0=x2, in1=C3, op=ALU.mult), reads=[bg, bcs], writes=[bt_])
                            self.op("pool", lambda e: e.tensor_tensor(out=u3[:, :, 64:128], in0=x1, in1=S3, op=ALU.mult), reads=[bg, bcs], writes=[bt_])
                            self.op("dve", lambda e: e.tensor_tensor(out=qr3[:, :, 64:128], in0=u3[:, :, 0:64], in1=u3[:, :, 64:128], op=ALU.add),
                                    reads=[bt_], writes=[bqr])
                        else:
                            self.op("dve", lambda e, q3=q3: e.tensor_tensor(out=qr3, in0=q3, in1=gn3, op=ALU.mult), reads=[bg, bcst], writes=[bqr])
                        pt, bpt = self.PS[j % 2]
                        ptb = pt

                        def em(e, qr=qr, ptb=ptb):
                            r = None
                            for h in range(nh):
                                r = e.transpose(out=ptb[:, h * 128:(h + 1) * 128], in_=qr[:, h * 128:(h + 1) * 128], identity=self.identb[:])
                            return r
                        self.op("pe", em, reads=[bqr, self.bconst], writes=[bpt])
                        o, bo = eb.next()
                        self.op("act", lambda e, o=o, ptb=ptb: e.activation(out=o[:, 0:w_], in_=ptb[:, 0:w_], func=AF.Copy), reads=[bpt], writes=[bo])
                        self.store(dst[:, :, t0 + j * 128:t0 + (j + 1) * 128].rearrange("h d t -> d h t"),
                                   o[:, 0:w_].rearrange("p (h t) -> p h t", h=nh), [bo])

                if CUT < 8:
                    continue
                wt, bwt = getpanel(4112, 512)
                if not (last and which == 1):
                    qk_path(wt, bwt, 0, 4, qg, Z["AQT"])
                wt, bwt = getpanel(4624, 512)
                qk_path(wt, bwt, 0, 2, kg, Z["AKT"])
                tm_simple(wt, bwt, 256, 256, Z["AV"])
            S.barrier()

    def phaseB(self, l, last):
        nc, S, I, Z = self.nc, self.S, self.I, self.Z
        with contextlib.ExitStack() as st:
            ngb, bngb = self.sb(st, "ngb", [128, 512], F32)
            self.load(ngb[:], I["ml_norm_g"][l].partition_broadcast(128), [bngb])
            gr = self.ring(st, "gates", [128, 16], F32, 3)
            sc = self.ring(st, "mlsc", [128, 32], F32, 3)
            kTr = self.ring(st, "kT4", [128, 4, 128], BF16, 3)
            qTr = self.ring(st, "qT4", [128, 4, 128], BF16, 3)
            kkr = self.ring(st, "kk", [128, 512], BF16, 3)
            ver = self.ring(st, "vext", [128, 4, 130], BF16, 3)
            vpr = self.ring(st, "vp", [128, 4, 130], BF16, 3)
            for (ve, bve) in ver.tiles:
                self.op("pool", lambda e, ve=ve: e.memset(ve[:], 1.0), writes=[bve])
            sTr = self.ring(st, "sT", [128, 128], BF16, 4)
            hbr = self.ring(st, "hbuf", [128, 512], F32, 3)
            hfr = self.ring(st, "hfl", [128, 512], F32, 2)
            mor = self.ring(st, "mo", [128, 512], BF16, 2)
            yar = self.ring(st, "ya", [128, 512], BF16, 2)
            yor = self.ring(st, "yo", [128, 512], BF16, 2)
            smr = self.ring(st, "smB", [128, 16], F32, 6)
            Cst = [[self.sb(st, "Cst", [128, 129], F32) for h in range(4)] for d in range(2)]
            Cb = [[self.sb(st, "Cb", [128, 129], BF16) for h in range(4)] for d in range(2)]
            psA = Ring(self.PS[2:4])
            psN = Ring(self.PS[4:6])
            psC = Ring(self.PS[6:8])
            LNS = float(np.log(128.0 ** -0.5))
            for d in range(2):
                for h in range(4):
                    c_, bc_ = Cst[d][h]
                    self.op("pool", lambda e, c_=c_: e.memset(c_[:], 0.0), writes=[bc_])
                    cb_, bcb_ = Cb[d][h]
                    self.op("pool", lambda e, cb_=cb_: e.memset(cb_[:], 0.0), writes=[bcb_])
                order = list(range(NT)) if d == 0 else [1, 0] + list(range(NT - 1, 1, -1))
                mk = self.mkF if d == 0 else self.mkB
                for c in order:
                    need_h = not (last and c < 2)
                    tsl = slice(c * 128, (c + 1) * 128)
                    g, bg = gr.next()
                    self.load(g[:], Z["MG"][tsl, :], [bg])
                    s_, bs_ = sc.next()
                    self.op("act", lambda e: e.activation(out=s_[:, 0:4], in_=g[:, d * 8 + 4:d * 8 + 8], func=AF.Exp, scale=-1.0), reads=[bg], writes=[bs_])
                    self.op("act", lambda e: e.activation(out=s_[:, 0:4], in_=s_[:, 0:4], func=AF.Ln, bias=1.0), reads=[bs_], writes=[bs_])
                    pc, bpc = psC.next()

                    def em(e):
                        e.matmul(pc[:, 0:4], lhsT=mk[:], rhs=s_[:, 0:4], start=True, stop=True)
                        return e.matmul(pc[:, 8:12], lhsT=self.onesf[:], rhs=s_[:, 0:4], start=True, stop=True)
                    self.op("pe", em, reads=[bs_, self.bconst], writes=[bpc])
                    self.op("dve", lambda e: e.tensor_tensor(out=s_[:, 4:8], in0=pc[:, 0:4], in1=g[:, d * 8:d * 8 + 4], op=ALU.add), reads=[bpc, bg], writes=[bs_])
                    self.op("dve", lambda e: e.tensor_scalar(out=s_[:, 4:8], in0=s_[:, 4:8], scalar1=LNS, scalar2=None, op0=ALU.add), reads=[bs_], writes=[bs_])
                    self.op("act", lambda e: e.activation(out=s_[:, 4:8], in_=s_[:, 4:8], func=AF.Exp), reads=[bs_], writes=[bs_])
                    self.op("act", lambda e: e.activation(out=s_[:, 8:12], in_=pc[:, 0:4], func=AF.Exp), reads=[bpc], writes=[bs_])
                    self.op("act", lambda e: e.activation(out=s_[:, 12:16], in_=pc[:, 8:12], func=AF.Exp, scale=-1.0), reads=[bpc], writes=[bs_])
                    kT, bkT = kTr.next()
                    self.load(kT[:], Z["MKT"][:, :, tsl].rearrange("h d t -> d h t"), [bkT])
                    kk, bkk = kkr.next()
                    self.load(kk[:], Z["MK"][tsl, :], [bkk])
                    ve, bve = ver.next()
                    self.load(ve[:, :, 0:128], Z["MV"][tsl, :].rearrange("t (h e) -> t h e", h=4), [bve])
                    vp, bvp = vpr.next()
                    if need_h:
                        qT, bqT = qTr.next()
                        self.load(qT[:], Z["MQT"][:, :, tsl].rearrange("h d t -> d h t"), [bqT])
                        hb, bhb = hbr.next()
                    for h in range(4):
                        c_, bc_ = Cst[d][h]
                        cb_, bcb_ = Cb[d][h]
                        self.op("dve", lambda e: e.tensor_scalar(out=vp[:, h, 0:129], in0=ve[:, h, 0:129], scalar1=s_[:, 4 + h:5 + h], scalar2=None, op0=ALU.mult),
                                reads=[bve, bs_], writes=[bvp])
                        if need_h:
                            pa, bpa = psA.next()
                            self.op("pe", lambda e: e.matmul(pa[:, 0:128], lhsT=kT[:, h, :], rhs=qT[:, h, :], start=True, stop=True), reads=[bkT, bqT], writes=[bpa])
                            sT, bsT = sTr.next()
                            self.op("dve", lambda e: e.scalar_tensor_tensor(out=sT[:], in0=pa[:, 0:128], scalar=s_[:, 4 + h:5 + h], in1=mk[:], op0=ALU.mult, op1=ALU.mult),
                                    reads=[bpa, bs_, self.bconst], writes=[bsT])
                            pn, bpn = psN.next()

                            def em(e):
                                e.matmul(pn[:, 0:129], lhsT=qT[:, h, :], rhs=cb_[:], start=True, stop=False)
                                return e.matmul(pn[:, 0:129], lhsT=sT[:], rhs=ve[:, h, 0:129], start=False, stop=True)
                            self.op("pe", em, reads=[bqT, bcb_, bsT, bve], writes=[bpn])
                            r_, br_ = smr.next()
                            self.op("act", lambda e: e.activation(out=r_[:, 0:1], in_=pn[:, 128:129], func=AF.Abs), reads=[bpn], writes=[br_])
                            self.op("dve", lambda e: e.tensor_tensor(out=r_[:, 0:1], in0=r_[:, 0:1], in1=s_[:, 8 + h:9 + h], op=ALU.max),
                                    reads=[br_, bs_], writes=[br_])
                            self.op("dve", lambda e: e.reciprocal(out=r_[:, 1:2], in_=r_[:, 0:1]), reads=[br_], writes=[br_])
                            self.op("act", lambda e: e.activation(out=hb[:, h * 128:(h + 1) * 128], in_=pn[:, 0:128], func=AF.Identity, scale=r_[:, 1:2]),
                                    reads=[bpn, br_], writes=[bhb])
                        pc2, bpc2 = psC.next()
                        self.op("pe", lambda e: e.matmul(pc2[:, 0:129], lhsT=kk[:, h * 128:(h + 1) * 128], rhs=vp[:, h, 0:129], start=True, stop=True), reads=[bkk, bvp], writes=[bpc2])
                        self.op("dve", lambda e: e.tensor_tensor(out=c_[:], in0=pc2[:, 0:129], in1=c_[:], op=ALU.add), reads=[bpc2, bc_], writes=[bc_])
                        self.op("act", lambda e: e.activation(out=c_[:], in_=c_[:], func=AF.Identity, scale=s_[:, 12 + h:13 + h]), reads=[bc_, bs_], writes=[bc_])
                        self.op("pool", lambda e: e.tensor_copy(out=cb_[:], in_=c_[:]), reads=[bc_], writes=[bcb_])
                    if not need_h:
                        continue
                    if d == 0:
                        self.store(Z["HF"][tsl, :], hb[:], [bhb])
                        continue
                    hf, bhf = hfr.next()
                    self.load(hf[:], Z["HF"][tsl, :], [bhf])
                    mo, bmo = mor.next()
                    self.load(mo[:], Z["MO"][tsl, :], [bmo])
                    self.op("dve", lambda e: e.tensor_tensor(out=hb[:], in0=hb[:], in1=hf[:], op=ALU.add), reads=[bhb, bhf], writes=[bhb])
                    ya, bya = yar.next()
                    for h in range(4):
                        hs = slice(h * 128, (h + 1) * 128)
                        r_, br_ = smr.next()
                        self.op("dve", lambda e: e.bn_stats(out=r_[:, 0:6], in_=hb[:, hs]), reads=[bhb], writes=[br_])
                        self.op("dve", lambda e: e.bn_aggr(out=r_[:, 8:10], in_=r_[:, 0:6]), reads=[br_], writes=[br_])
                        self.op("act", lambda e: e.activation(out=r_[:, 10:11], in_=r_[:, 9:10], func=AF.Sqrt, scale=1.0, bias=EPS), reads=[br_], writes=[br_])
                        self.op("dve", lambda e: e.reciprocal(out=r_[:, 11:12], in_=r_[:, 10:11]), reads=[br_], writes=[br_])
                        self.op("dve", lambda e: e.tensor_scalar(out=hb[:, hs], in0=hb[:, hs], scalar1=r_[:, 8:9], scalar2=r_[:, 11:12], op0=ALU.subtract, op1=ALU.mult),
                                reads=[bhb, br_], writes=[bhb])
                    self.op("pool", lambda e: e.tensor_tensor(out=hb[:], in0=hb[:], in1=ngb[:], op=ALU.mult), reads=[bhb, bngb], writes=[bhb])
                    self.op("dve", lambda e: e.tensor_tensor(out=ya[:], in0=hb[:], in1=mo[:], op=ALU.mult), reads=[bhb, bmo], writes=[bya])
                    pt, bpt = self.PS[c % 2]

                    def em(e):
                        r = None
                        for h in range(4):
                            r = e.transpose(out=pt[:, h * 128:(h + 1) * 128], in_=ya[:, h * 128:(h + 1) * 128], identity=self.identb[:])
                        return r
                    self.op("pe", em, reads=[bya, self.bconst], writes=[bpt])
                    yo, byo = yor.next()
                    self.op("act", lambda e: e.activation(out=yo[:], in_=pt[:, 0:512], func=AF.Copy), reads=[bpt], writes=[byo])
                    self.store(Z["YT"][0:4, :, tsl].rearrange("h d t -> d h t"), yo[:].rearrange("p (h t) -> p h t", h=4), [byo])
            S.barrier()

    def phaseC(self, l, last):
        nc, S, I, Z = self.nc, self.S, self.I, self.Z
        with contextlib.ExitStack() as st:
            wsT, bws = self.sb(st, "wsT", [128, 4, 128], BF16)
            wst, bwst = self.sb(st, "wstg", [128, 128], F32)
            for g in range(4):
                self.load(wst[:], I["sg_w"][l, g], [bwst])
                ps, bps = self.PS[7]
                self.op("pe", lambda e: e.transpose(out=ps[:, 0:128], in_=wst[:], identity=self.ident[:]), reads=[bwst, self.bconst], writes=[bps])
                self.op("dve", lambda e: e.tensor_copy(out=wsT[:, g, :], in_=ps[:, 0:128]), reads=[bps], writes=[bws])
            bsf, bbsf = self.sb(st, "bsf", [1, 512], F32)
            self.load(bsf[:], I["sg_b"][l:l + 1].rearrange("o g t -> o (g t)"), [bbsf])
            bsb, bbsb = self.sb(st, "bsb", [1, 512], BF16)
            self.op("dve", lambda e: e.tensor_copy(out=bsb[:], in_=bsf[:]), reads=[bbsf], writes=[bbsb])
            svr = self.ring(st, "sv", [128, 512], BF16, 3)
            sur = self.ring(st, "su", [128, 4, 128], BF16, 3)
            ybr = self.ring(st, "yb", [128, 4, 128], BF16, 3)
            psr = Ring(self.PS[2:7])
            for c in range(2 if last else 0, NT):
                tsl = slice(c * 128, (c + 1) * 128)
                sv, bsv = svr.next()
                self.load(sv[:], Z["SV"][tsl, :], [bsv])
                su, bsu = sur.next()
                self.load(su[:], Z["SU"][:, :, tsl].rearrange("g c t -> c g t"), [bsu])
                yb, byb = ybr.next()
                for g in range(4):
                    ps, bps = psr.next()

                    def em(e):
                        e.matmul(ps[:, 0:128], lhsT=sv[:, g * 128:(g + 1) * 128], rhs=wsT[:, g, :], start=True, stop=False)
                        return e.matmul(ps[:, 0:128], lhsT=self.onesb[0:1, :], rhs=bsb[0:1, g * 128:(g + 1) * 128], start=False, stop=True)
                    self.op("pe", em, reads=[bsv, bws, bbsb, self.bconst], writes=[bps])
                    self.op("dve", lambda e: e.tensor_tensor(out=yb[:, g, :], in0=ps[:, 0:128], in1=su[:, g, :], op=ALU.mult), reads=[bps, bsu], writes=[byb])
                self.store(Z["YT"][4:8, :, tsl].rearrange("g c t -> c g t"), yb[:], [byb])
            S.barrier()

    def phaseD(self, l, last):
        nc, S, I, Z = self.nc, self.S, self.I, self.Z
        with contextlib.ExitStack() as st:
            wfm, bwfm = self.sb(st, "cvw", [128, 124 + 12], F32)
            self.load_fm(st, wfm[:, 0:124], bwfm, I["cv_w"][l].rearrange("k (c p) -> (k c) p", p=128), 124)
            self.load_fm(st, wfm[:, 124:128], bwfm, I["cv_b"][l].rearrange("(c p) -> c p", p=128), 4)
            self.load_fm(st, wfm[:, 128:132], bwfm, I["cv_ln_g"][l].rearrange("(c p) -> c p", p=128), 4)
            self.load_fm(st, wfm[:, 132:136], bwfm, I["cv_ln_b"][l].rearrange("(c p) -> c p", p=128), 4)
            dg, bdg = self.sb(st, "diag", [128, 124, 128], F32)
            for r in range(124):
                eng = "dve"
                self.op(eng, lambda e: e.tensor_scalar(out=dg[:, r, :], in0=self.ident[:], scalar1=wfm[:, r:r + 1], scalar2=None, op0=ALU.mult),
                        reads=[bwfm, self.bconst], writes=[bdg])
            ybr = [self.ring(st, "cyb", [128, 542], F32, 2) for ch in range(4)]
            y2r = [self.ring(st, "y2", [128, 512], F32, 2) for ch in range(4)]
            sqr = self.ring(st, "sq", [128, 512], F32, 2)
            str_ = self.ring(st, "stt", [128, 512], F32, 6)
            yor = self.ring(st, "cyo", [128, 512], BF16, 3)
            seqs = ([] if last else [(0, TC)]) + [(TC, T)]
            for (s0, s1) in seqs:
                for t0 in range(s0, s1, 512):
                    n = min(512, s1 - t0)
                    lo = max(s0, t0 - 15)
                    hi = min(s1, t0 + n + 15)
                    y2s = []
                    for ch in range(4):
                        yb, byb = ybr[ch].next()
                        if lo > t0 - 15:
                            self.op("pool", lambda e: e.memset(yb[:, 0:15], 0.0), writes=[byb])
                        if hi < t0 + n + 15:
                            self.op("pool", lambda e: e.memset(yb[:, n + 15:n + 30], 0.0), writes=[byb])
                        self.load(yb[:, lo - (t0 - 15):hi - (t0 - 15)], Z["CY"][ch, :, lo:hi], [byb])
                        ps, bps = self.PS[2 + ch]

                        def em(e):
                            r = None
                            for k in range(31):
                                r = e.matmul(ps[:, 0:n], lhsT=dg[:, k * 4 + ch, :], rhs=yb[:, k:k + n], start=(k == 0), stop=(k == 30))
                            return r
                        self.op("pe", em, reads=[bdg, byb], writes=[bps])
                        y2, by2 = y2r[ch].next()
                        self.op("act", lambda e: e.activation(out=y2[:, 0:n], in_=ps[:, 0:n], func=AF.Identity, bias=wfm[:, 124 + ch:125 + ch], scale=1.0), reads=[bps, bwfm], writes=[by2])
                        y2s.append((y2, by2))
                    p1, bp1 = self.PS[6]
                    p2, bp2 = self.PS[7]

                    def em(e):
                        r = None
                        for ch in range(4):
                            r = e.matmul(p1[:, 0:n], lhsT=self.onesf[:], rhs=y2s[ch][0][:, 0:n], start=(ch == 0), stop=(ch == 3))
                        return r
                    self.op("pe", em, reads=[b for (_, b) in y2s] + [self.bconst], writes=[bp1])
                    sqs = []
                    for ch in range(4):
                        sq, bsq = sqr.next()
                        self.op("act", lambda e: e.activation(out=sq[:, 0:n], in_=y2s[ch][0][:, 0:n], func=AF.Square), reads=[y2s[ch][1]], writes=[bsq])
                        self.op("pe", lambda e: e.matmul(p2[:, 0:n], lhsT=self.onesf[:], rhs=sq[:, 0:n], start=(ch == 0), stop=(ch == 3)), reads=[bsq, self.bconst], writes=[bp2])
                    mean, bmean = str_.next()
                    self.op("dve", lambda e: e.tensor_scalar(out=mean[:, 0:n], in0=p1[:, 0:n], scalar1=1.0 / 512, scalar2=None, op0=ALU.mult), reads=[bp1], writes=[bmean])
                    m2, bm2 = str_.next()
                    self.op("pool", lambda e: e.tensor_tensor(out=m2[:, 0:n], in0=mean[:, 0:n], in1=mean[:, 0:n], op=ALU.mult), reads=[bmean], writes=[bm2])
                    var, bvar = str_.next()
                    self.op("dve", lambda e: e.scalar_tensor_tensor(out=var[:, 0:n], in0=p2[:, 0:n], scalar=1.0 / 512, in1=m2[:, 0:n], op0=ALU.mult, op1=ALU.subtract),
                            reads=[bp2, bm2], writes=[bvar])
                    self.op("act", lambda e: e.activation(out=var[:, 0:n], in_=var[:, 0:n], func=AF.Sqrt, scale=1.0, bias=EPS), reads=[bvar], writes=[bvar])
                    self.op("dve", lambda e: e.reciprocal(out=var[:, 0:n], in_=var[:, 0:n]), reads=[bvar], writes=[bvar])
                    for ch in range(4):
                        y2, by2 = y2s[ch]
                        self.op("dve", lambda e: e.tensor_tensor(out=y2[:, 0:n], in0=y2[:, 0:n], in1=mean[:, 0:n], op=ALU.subtract), reads=[by2, bmean], writes=[by2])
                        self.op("pool", lambda e: e.tensor_tensor(out=y2[:, 0:n], in0=y2[:, 0:n], in1=var[:, 0:n], op=ALU.mult), reads=[by2, bvar], writes=[by2])
                        yo, byo = yor.next()
                        self.op("act", lambda e: e.activation(out=yo[:, 0:n], in_=y2[:, 0:n], func=AF.Silu, scale=wfm[:, 128 + ch:129 + ch], bias=wfm[:, 132 + ch:133 + ch]),
                                reads=[by2, bwfm], writes=[byo])
                        self.store(Z["YT"][8 + ch, :, t0:t0 + n], yo[:, 0:n], [byo])
            S.barrier()

    def phaseE(self, l, last):
        nc, S, I, Z = self.nc, self.S, self.I, self.Z
        with contextlib.ExitStack() as st:
            akt, bakt = self.sb(st, "akt", [128, 2, T], BF16)
            self.load(akt[:], Z["AKT"].rearrange("h d t -> d h t"), [bakt])
            av, bav = self.sb(st, "av", [128, NT, 256], BF16)
            self.load(av[:], Z["AV"].rearrange("(n p) c -> p n c", p=128), [bav])
            gq, bgq = self.sb(st, "gq", [128, 260], F32)
            self.load(gq[:, 0:128], I["at_qn_g"][l].partition_broadcast(128), [bgq])
            self.load(gq[:, 128:256], I["at_kn_g"][l].partition_broadcast(128), [bgq])
            self.op("dve", lambda e: e.tensor_reduce(out=gq[:, 256:257], in_=gq[:, 0:128], axis=AX.X, op=ALU.max, apply_absolute_value=True), reads=[bgq], writes=[bgq])
            self.op("dve", lambda e: e.tensor_reduce(out=gq[:, 257:258], in_=gq[:, 128:256], axis=AX.X, op=ALU.max, apply_absolute_value=True), reads=[bgq], writes=[bgq])
            self.op("dve", lambda e: e.scalar_tensor_tensor(out=gq[:, 258:259], in0=gq[:, 256:257], scalar=-float(128.0 ** 0.5), in1=gq[:, 257:258], op0=ALU.mult, op1=ALU.mult),
                    reads=[bgq], writes=[bgq])
            negC = gq[:, 258:259]
            qr = self.ring(st, "aq", [128, 512], BF16, 2)
            pr = self.ring(st, "ap", [128, 512], BF16, 4)
            rsr = self.ring(st, "ars", [128, 512], F32, 2)
            yor = self.ring(st, "ayo", [128, 512], BF16, 2)
            psS = Ring(self.PS[2:6])
            acc = Ring([(self.PS[6], self.PS[7])])
            SCL = float(128.0 ** -0.5)
            jobs = []
            if not last:
                for h in range(4):
                    jobs.append((h, 0, 256, [0, 1]))
            for h in range(4):
                for tc in range(TL // 512):
                    jobs.append((h, TC + tc * 512, 512, list(range(NT))))
            for (h, t0, n, stiles) in jobs:
                kvh = h // 2
                q, bq = qr.next()
                self.load(q[:, 0:n], Z["AQT"][h, :, t0:t0 + n], [bq])
                (po, bpo), (psm, bpsm) = self.PS[6], self.PS[7]
                for i, s in enumerate(stiles):
                    ps, bps = psS.next()
                    self.op("pe", lambda e: e.matmul(ps[:, 0:n], lhsT=akt[:, kvh, s * 128:(s + 1) * 128], rhs=q[:, 0:n], start=True, stop=True), reads=[bakt, bq], writes=[bps])
                    p, bp = pr.next()
                    self.op("act", lambda e: e.activation(out=p[:, 0:n], in_=ps[:, 0:n], func=AF.Exp, scale=SCL, bias=negC), reads=[bps, bgq], writes=[bp])
                    first, lastk = (i == 0), (i == len(stiles) - 1)

                    def em(e):
                        e.matmul(po[:, 0:n], lhsT=av[:, s, kvh * 128:(kvh + 1) * 128], rhs=p[:, 0:n], start=first, stop=lastk)
                        return e.matmul(psm[:, 0:n], lhsT=self.onesb[:], rhs=p[:, 0:n], start=first, stop=lastk)
                    self.op("pe", em, reads=[bav, bp, self.bconst], writes=[bpo, bpsm])
                rs, brs = rsr.next()
                self.op("dve", lambda e: e.reciprocal(out=rs[:, 0:n], in_=psm[:, 0:n]), reads=[bpsm], writes=[brs])
                yo, byo = yor.next()
                self.op("dve", lambda e: e.tensor_tensor(out=yo[:, 0:n], in0=po[:, 0:n], in1=rs[:, 0:n], op=ALU.mult), reads=[bpo, brs], writes=[byo])
                self.store(Z["YT"][12 + h, :, t0:t0 + n], yo[:, 0:n], [byo])
            S.barrier()

    def phaseF(self, l, xsrc, last):
        nc, S, I, Z = self.nc, self.S, self.I, self.Z
        with contextlib.ExitStack() as st:
            wo, bwo = self.sb(st, "wo", [128, KC, D], BF16)
            wsrc = I["w_out"][l].rearrange("(k p) c -> p k c", p=128)
            for q in range(4):
                self.load(wo[:, :, q * 512:(q + 1) * 512], wsrc[:, :, q * 512:(q + 1) * 512], [bwo], q="pool")
            G, bG = self.sb(st, "G1", [128, 2, D], F32)
            for w in range(2):
                self.load(G[:, w, :], Z["GV"][l, w, 0].partition_broadcast(128), [bG])
            yr = self.ring(st, "yT", [128, KC, 128], BF16, 3)
            xr = self.ring(st, "xF", [128, D], F32, 3)
            orr = self.ring(st, "oF", [128, D], F32, 2)
            jr = self.ring(st, "jF", [128, 512], BF16, 2)
            smr = self.ring(st, "smF", [128, 8], F32, 3)
            for c in range(2 if last else 0, NT):
                which = 1 if c < 2 else 0
                tsl = slice(c * 128, (c + 1) * 128)
                y, by = yr.next()
                self.load(y[:], Z["YT"][:, :, tsl].rearrange("k p t -> p k t"), [by])
                x, bx = xr.next()
                self.load(x[:], xsrc[tsl, :], [bx])
                sm, bsm = smr.next()
                for dc in range(4):
                    ps, bps = self.PS[2 + dc]

                    def em(e):
                        r = None
                        for kc in range(KC):
                            r = e.matmul(ps[:], lhsT=y[:, kc, :], rhs=wo[:, kc, dc * 512:(dc + 1) * 512], start=(kc == 0), stop=(kc == KC - 1))
                        return r
                    self.op("pe", em, reads=[by, bwo], writes=[bps])
                    j_, bj_ = jr.next()
                    self.op("act", lambda e: e.activation(out=j_[:], in_=ps[:], func=AF.Square, accum_out=sm[:, dc:dc + 1]), reads=[bps], writes=[bj_, bsm])
                self.op("dve", lambda e: e.tensor_reduce(out=sm[:, 4:5], in_=sm[:, 0:4], axis=AX.X, op=ALU.add), reads=[bsm], writes=[bsm])
                self.op("act", lambda e: e.activation(out=sm[:, 5:6], in_=sm[:, 4:5], func=AF.Sqrt, scale=1.0 / D, bias=EPS), reads=[bsm], writes=[bsm])
                self.op("dve", lambda e: e.reciprocal(out=sm[:, 6:7], in_=sm[:, 5:6]), reads=[bsm], writes=[bsm])
                o, bo = orr.next()
                for dc in range(4):
                    ps, bps = self.PS[2 + dc]
                    dsl = slice(dc * 512, (dc + 1) * 512)
                    self.op("dve", lambda e: e.scalar_tensor_tensor(out=o[:, dsl], in0=ps[:], scalar=sm[:, 6:7], in1=G[:, which, dsl], op0=ALU.mult, op1=ALU.mult),
                            reads=[bps, bsm, bG], writes=[bo])
                self.op("pool", lambda e: e.tensor_tensor(out=o[:], in0=o[:], in1=x[:], op=ALU.add), reads=[bo, bx], writes=[bo])
                self.store(Z["XA"][tsl, :], o[:], [bo])
            S.barrier()

    def phaseG(self, l, last):
        nc, S, I, Z = self.nc, self.S, self.I, self.Z
        TBG = 512
        with contextlib.ExitStack() as st:
            cw, bcw = self.sb(st, "fcw", [128, 132 + 44], F32)
            src = I["ffn_cv_w"][l].rearrange("k (f p) -> (k f) p", p=128)
            self.load_fm(st, cw[:, 0:128], bcw, src[0:128, :], 128)
            self.load_fm(st, cw[:, 128:132], bcw, src[128:132, :], 4)
            self.load_fm(st, cw[:, 132:176], bcw, I["ffn_cv_b"][l].rearrange("(f p) -> f p", p=128), 44)
            G, bG = self.sb(st, "G2", [128, 2, D], F32)
            for w in range(2):
                self.load(G[:, w, :], Z["GV"][l, w, 1].partition_broadcast(128), [bG])
            hT, _ = self.sb(st, "hT2", [128, KC, TBG + 128], BF16)
            bh = [(Buf(), Buf()) for _ in range(TBG // 128 + 1)]
            aT, _ = self.sb(st, "aT", [128, FC, TBG], BF16)
            ba = [Buf() for _ in range(FC)]
            xring = self.ring(st, "xg", [128, D], F32, 3)
            nring = self.ring(st, "xng", [128, D], BF16, 2)
            rring = self.ring(st, "rsg", [128, 4], F32, 3)
            wur = self.ring(st, "wu", [128, KC, 256], BF16, 2)
            wdr = self.ring(st, "wd", [128, FC, 256], BF16, 2)
            gbr = self.ring(st, "gbuf", [128, TBG + 2], F32, 2)
            tr_ = self.ring(st, "tg", [128, TBG], F32, 3)
            er = self.ring(st, "eg", [128, 256], F32, 3)
            jr = self.ring(st, "jg", [128, 256], BF16, 2)
            ssq, bssq = self.sb(st, "ssq", [128, 32], F32)
            wusrc = I["w_up"][l].rearrange("(k p) c -> p k c", p=128)
            wdsrc = I["w_down"][l].rearrange("(f p) d -> p f d", p=128)
            psg = Ring(self.PS[2:6])
            seqs = ([] if last else [(0, TC, 1)]) + [(TC, T, 0)]
            nblk = 0
            for (s0, s1, which) in seqs:
                for t0 in range(s0, s1, TBG):
                    n = min(TBG, s1 - t0)
                    ntl = n // 128
                    first = (nblk == 0)
                    nblk += 1
                    GC = int(os.environ.get("GCUT", "9"))
                    if GC < 2:
                        continue
                    self.prenorm_T(Z["XA"], t0, ntl, hT, bh, 0, l, which, xring, nring, rring, "2")
                    self._halo = (t0 - 1 if t0 - 1 >= s0 else None, t0 + n if t0 + n < s1 else None)
                    self.prenorm_T(Z["XA"], t0, 1, hT, bh[ntl:ntl + 1], TBG, l, which, xring, nring, rring, "2", halo=self._halo)
                    hb_all = [b for j in range(ntl) for b in bh[j]]
                    if GC < 3:
                        continue
                    for f in range(FC):
                        wu, bwu = wur.next()
                        if first:
                            self.load(wu[:, :, 0:128], wusrc[:, :, f * 128:(f + 1) * 128], [bwu], q="pool")
                            self.load(wu[:, :, 128:256], wusrc[:, :, DFF + f * 128:DFF + (f + 1) * 128], [bwu], q="pool")
                            self.store(Z["WUS"][f], wu[:].rearrange("p k c -> p (k c)"), [bwu])
                        else:
                            self.load(wu[:].rearrange("p k c -> p (k c)"), Z["WUS"][f], [bwu])
                        pg, bpg = psg.next()
                        pu, bpu = psg.next()
                        ph, bph = self.PS[6]

                        def em(e):
                            r = None
                            for kc in range(KC):
                                r = e.matmul(pg[:, 0:n], lhsT=wu[:, kc, 0:128], rhs=hT[:, kc, 0:n], start=(kc == 0), stop=(kc == KC - 1))
                            return r
                        self.op("pe", em, reads=[bwu] + hb_all, writes=[bpg])

                        def em(e):
                            r = None
                            for kc in range(KC):
                                r = e.matmul(ph[:, 0:2], lhsT=wu[:, kc, 0:128], rhs=hT[:, kc, TBG:TBG + 2], start=(kc == 0), stop=(kc == KC - 1))
                            return r
                        self.op("pe", em, reads=[bwu, bh[ntl][0]], writes=[bph])

                        def em(e):
                            r = None
                            for kc in range(KC):
                                r = e.matmul(pu[:, 0:n], lhsT=wu[:, kc, 128:256], rhs=hT[:, kc, 0:n], start=(kc == 0), stop=(kc == KC - 1))
                            return r
                        self.op("pe", em, reads=[bwu] + hb_all, writes=[bpu])
                        gb_, bgb = gbr.next()
                        self.op("act", lambda e: e.activation(out=gb_[:, 1:n + 1], in_=pg[:, 0:n], func=AF.Copy), reads=[bpg], writes=[bgb])
                        if self._halo[0] is not None:
                            self.op("dve", lambda e: e.tensor_copy(out=gb_[:, 0:1], in_=ph[:, 0:1]), reads=[bph], writes=[bgb])
                        else:
                            self.op("dve", lambda e: e.memset(gb_[:, 0:1], 0.0), reads=[bph], writes=[bgb])
                        if self._halo[1] is not None:
                            self.op("dve", lambda e: e.tensor_copy(out=gb_[:, n + 1:n + 2], in_=ph[:, 1:2]), reads=[bph], writes=[bgb])
                        else:
                            self.op("dve", lambda e: e.memset(gb_[:, n + 1:n + 2], 0.0), reads=[bph], writes=[bgb])
                        t_, bt_ = tr_.next()
                        self.op("dve", lambda e: e.tensor_scalar(out=t_[:, 0:n], in0=gb_[:, 0:n], scalar1=cw[:, f:f + 1], scalar2=cw[:, 132 + f:133 + f], op0=ALU.mult, op1=ALU.add),
                                reads=[bgb, bcw], writes=[bt_])
                        self.op("dve", lambda e: e.scalar_tensor_tensor(out=t_[:, 0:n], in0=gb_[:, 1:n + 1], scalar=cw[:, 44 + f:45 + f], in1=t_[:, 0:n], op0=ALU.mult, op1=ALU.add),
                                reads=[bgb, bcw, bt_], writes=[bt_])
                        self.op("dve", lambda e: e.scalar_tensor_tensor(out=t_[:, 0:n], in0=gb_[:, 2:n + 2], scalar=cw[:, 88 + f:89 + f], in1=t_[:, 0:n], op0=ALU.mult, op1=ALU.add),
                                reads=[bgb, bcw, bt_], writes=[bt_])
                        self.op("act", lambda e: e.activation(out=t_[:, 0:n], in_=t_[:, 0:n], func=AF.Silu), reads=[bt_], writes=[bt_])
                        self.op("dve", lambda e: e.tensor_tensor(out=aT[:, f, 0:n], in0=pu[:, 0:n], in1=t_[:, 0:n], op=ALU.mult), reads=[bpu, bt_], writes=[ba[f]])
                    if GC < 4:
                        continue
                    for dc in range(D // 256):
                        wd, bwd = wdr.next()
                        if first:
                            for fq in range(4):
                                self.load(wd[:, fq * 11:(fq + 1) * 11, :], wdsrc[:, fq * 11:(fq + 1) * 11, dc * 256:(dc + 1) * 256], [bwd], q="pool")
                            self.store(Z["WDS"][dc], wd[:].rearrange("p f c -> p (f c)"), [bwd])
                        else:
                            self.load(wd[:].rearrange("p f c -> p (f c)"), Z["WDS"][dc], [bwd])
                        for tt in range(ntl):
                            ps, bps = self.PS[6 + (dc * ntl + tt) % 2]

                            def em(e):
                                r = None
                                for f in range(FC):
                                    r = e.matmul(ps[:, 0:256], lhsT=aT[:, f, tt * 128:(tt + 1) * 128], rhs=wd[:, f, :], start=(f == 0), stop=(f == FC - 1))
                                return r
                            self.op("pe", em, reads=[bwd] + ba, writes=[bps])
                            j_, bj_ = jr.next()
                            e_, be_ = er.next()
                            self.op("dve", lambda e: e.tensor_copy(out=e_[:], in_=ps[:, 0:256]), reads=[bps], writes=[be_])
                            self.op("act", lambda e: e.activation(out=j_[:], in_=e_[:], func=AF.Square, accum_out=ssq[:, tt * 8 + dc:tt * 8 + dc + 1]), reads=[be_], writes=[bj_, bssq])
                            if not os.environ.get("NOST"):
                                self.store(Z["RAW"][t0 + tt * 128:t0 + (tt + 1) * 128, dc * 256:(dc + 1) * 256], e_[:], [be_])
                    S.barrier()
                    if GC < 5:
                        continue
                    for tt in range(ntl):
                        tsl = slice(t0 + tt * 128, t0 + (tt + 1) * 128)
                        r_, br_ = rring.next()
                        self.op("dve", lambda e: e.tensor_reduce(out=r_[:, 0:1], in_=ssq[:, tt * 8:tt * 8 + 8], axis=AX.X, op=ALU.add), reads=[bssq], writes=[br_])
                        self.op("act", lambda e: e.activation(out=r_[:, 1:2], in_=r_[:, 0:1], func=AF.Sqrt, scale=1.0 / D, bias=EPS), reads=[br_], writes=[br_])
                        self.op("dve", lambda e: e.reciprocal(out=r_[:, 2:3], in_=r_[:, 1:2]), reads=[br_], writes=[br_])
                        raw, braw = xring.next()
                        self.load(raw[:], Z["RAW"][tsl, :], [braw])
                        x, bx = xring.next()
                        self.load(x[:], Z["XA"][tsl, :], [bx])
                        self.op("dve", lambda e: e.scalar_tensor_tensor(out=raw[:], in0=raw[:], scalar=r_[:, 2:3], in1=G[:, which, :], op0=ALU.mult, op1=ALU.mult),
                                reads=[braw, br_, bG], writes=[braw])
                        self.op("pool", lambda e: e.tensor_tensor(out=raw[:], in0=raw[:], in1=x[:], op=ALU.add), reads=[braw, bx], writes=[braw])
                        if last:
                            self.store(self.out[tsl.start - TC:tsl.stop - TC, :], raw[:], [braw])
                        else:
                            self.store(Z["XB"][tsl, :], raw[:], [braw])
            S.barrier()


def rope_table():
    t = np.arange(TL)
    row = (t // 64).astype(np.float32)
    col = (t % 64).astype(np.float32)
    inv = np.power(np.float32(10000.0), -np.arange(0, 64, 2, dtype=np.float32) / np.float32(64)).astype(np.float32)
    ang = np.concatenate([row[:, None] * inv, col[:, None] * inv], axis=-1).astype(np.float32)
    return np.ascontiguousarray(np.concatenate([np.cos(ang), np.sin(ang)], axis=-1).astype(np.float32))


PARAMS = ["w_ada", "b_ada", "pre_mix_g", "post_mix_g", "w_in", "ml_gate_b", "ml_norm_g", "sg_ln_g", "sg_ln_b", "sg_w",
          "sg_b", "cv_w", "cv_b", "cv_ln_g", "cv_ln_b", "at_qn_g", "at_kn_g", "w_out", "pre_ffn_g", "post_ffn_g", "w_up",
          "ffn_cv_w", "ffn_cv_b", "w_down"]


def make_in_map(inputs, b, rope):
    m = {"xin": np.ascontiguousarray(np.concatenate([inputs["ctx"][b], inputs["x"][b]], axis=0), dtype=np.float32),
         "cvec": np.ascontiguousarray(np.stack([inputs["c"][b], inputs["c_ctx"]], axis=0), dtype=np.float32),
         "rope": rope}
    for p in PARAMS:
        m[p] = np.ascontiguousarray(inputs[p], dtype=np.float32)
    return m


def kernel(**inputs):
    inputs = {k: np.asarray(v) for k, v in inputs.items()}
    kb = K()
    rope = rope_table()
    maps = [make_in_map(inputs, c % 4, rope) for c in range(NCORES)]
    res = run_bass_kernel_spmd(kb.nc, maps, core_ids=list(range(NCORES)))
    out = np.stack([np.asarray(res.results[b]["out"], dtype=np.float32) for b in range(4)], axis=0)
    return out
```
